# Optimizing a Trainium2 kernel written in Bass

```python
import jax, jax.numpy as jnp
from jax import lax
import numpy as np

D_MODEL = 2048
BATCH = 1
SEQ = 8192
DEPTH = 1

HEAD_DIM = 64
D_RWKV = D_MODEL // 2
D_ATTN = D_MODEL - D_RWKV
H_RWKV = D_RWKV // HEAD_DIM
H_ATTN = D_ATTN // HEAD_DIM
DECAY_LORA = 64
ICLR_LORA = 64
GATE_LORA = 160
GN_EPS = 64e-5
RMS_EPS = 1e-6
RWKV_SPLITS = (D_RWKV, 2 * D_RWKV, 3 * D_RWKV,
               3 * D_RWKV + DECAY_LORA, 3 * D_RWKV + 2 * DECAY_LORA,
               3 * D_RWKV + 2 * DECAY_LORA + ICLR_LORA,
               3 * D_RWKV + 2 * DECAY_LORA + 2 * ICLR_LORA)
RWKV_COLS = 3 * D_RWKV + 2 * DECAY_LORA + 2 * ICLR_LORA + GATE_LORA
ATTN_COLS = 3 * D_ATTN
N_IN = RWKV_COLS + ATTN_COLS
ROPE_THETA = 500000.0
ROPE_DIM = HEAD_DIM // 4
DILATED_PATTERNS = ((128, 1), (512, 4), (2048, 16))
Q_BLOCK = 128
N_GROUPS = 4
EXPERTS_PER_GROUP = 8
N_EXPERTS = N_GROUPS * EXPERTS_PER_GROUP
EXPERT_TOPK = 2
D_EXPERT = D_MODEL // 2
NEG_BIG = -1e30

kernel_name = "hymba_rwkv7_dilated_hmoe_encoder"


def rms_norm(x, g):
    xf = x.astype(jnp.float32)
    y = xf * lax.rsqrt(jnp.mean(xf * xf, axis=-1, keepdims=True) + RMS_EPS)
    return (y * g.astype(jnp.float32)).astype(x.dtype)


def centred_token_shift(z, mu_prev, mu_next):
    z_prev = jnp.pad(z[:, :-1], ((0, 0), (1, 0), (0, 0)))
    z_next = jnp.pad(z[:, 1:], ((0, 0), (0, 1), (0, 0)))
    return z + mu_prev * (z_prev - z) + mu_next * (z_next - z)


def wkv7_scan(r, w, k, v, a_vec, b_vec, reverse):
    B, S, H, N = r.shape

    def step(state, inp):
        r_t, w_t, k_t, v_t, a_t, b_t = inp
        sa = jnp.einsum('bhvk,bhk->bhv', state, a_t)
        state = (state * w_t[:, :, None, :] + sa[..., None] * b_t[:, :, None, :]
                 + v_t[..., None] * k_t[:, :, None, :])
        y_t = jnp.einsum('bhvk,bhk->bhv', state, r_t)
        return state, y_t

    xs = tuple(jnp.moveaxis(t, 1, 0) for t in (r, w, k, v, a_vec, b_vec))
    s0 = jnp.zeros((B, H, N, N), jnp.float32)
    _, ys = lax.scan(step, s0, xs, reverse=reverse)
    return jnp.moveaxis(ys, 0, 1)


def rwkv7_mixer(z, mu, w0, w2, a0, a2, g2, k_k, k_a, r_k, ln_w, ln_b):
    B, S, _ = z.shape
    out_dtype = z.dtype
    z = centred_token_shift(z, mu[0], mu[1]).astype(jnp.float32)
    r, k, v, wl_f, wl_b, al_f, al_b, gl = jnp.split(z, list(RWKV_SPLITS), axis=-1)

    def heads(t):
        return t.reshape(B, S, H_RWKV, HEAD_DIM)

    kk = heads(k * k_k)
    kk = kk * lax.rsqrt(jnp.sum(kk * kk, axis=-1, keepdims=True) + 1e-12)
    rh, vh = heads(r), heads(v)
    y = jnp.zeros((B, S, H_RWKV, HEAD_DIM), jnp.float32)
    for d, (wl, al, rev) in enumerate(((wl_f, al_f, False), (wl_b, al_b, True))):
        w_log = -jax.nn.softplus(-(w0[d] + jnp.tanh(wl) @ w2[d])) - 0.5
        decay = jnp.exp(-jnp.exp(w_log))
        a = jax.nn.sigmoid(a0[d] + al @ a2[d])
        k_d = k * (1.0 + (a - 1.0) * k_a)
        ah = heads(a)
        y = y + wkv7_scan(rh, heads(decay), heads(k_d), vh, -kk, kk * ah, rev)
    mean = jnp.mean(y, axis=-1, keepdims=True)
    var = jnp.mean(jnp.square(y - mean), axis=-1, keepdims=True)
    yn = ((y - mean) * lax.rsqrt(var + GN_EPS)).reshape(B, S, D_RWKV) * ln_w + ln_b
    bonus = (jnp.sum(rh * heads(k) * r_k, axis=-1, keepdims=True) * vh).reshape(B, S, D_RWKV)
    g = jax.nn.sigmoid(gl) @ g2
    return ((yn + bonus) * g).astype(out_dtype)


def partial_rotary(x, positions):
    half = ROPE_DIM // 2
    inv_freq = jnp.power(ROPE_THETA, -jnp.arange(half, dtype=jnp.float32) * 2.0 / ROPE_DIM)
    ang = positions.astype(jnp.float32)[..., None] * inv_freq
    cos = jnp.cos(ang)[:, :, None, :]
    sin = jnp.sin(ang)[:, :, None, :]
    xf = x.astype(jnp.float32)
    x1 = xf[..., :half]
    x2 = xf[..., half:ROPE_DIM]
    out = jnp.concatenate([x1 * cos - x2 * sin, x2 * cos + x1 * sin, xf[..., ROPE_DIM:]], axis=-1)
    return out.astype(x.dtype)


def banded_attention(q, k, v, radius):
    N, L, Dh = q.shape
    nb = -(-L // Q_BLOCK)
    Lp = nb * Q_BLOCK
    kb_len = Q_BLOCK + 2 * radius
    pad = Lp - L
    qb = jnp.pad(q, ((0, 0), (0, pad), (0, 0))).reshape(N, nb, Q_BLOCK, Dh)
    kp = jnp.pad(k, ((0, 0), (radius, pad + radius), (0, 0)))
    vp = jnp.pad(v, ((0, 0), (radius, pad + radius), (0, 0)))
    key_idx = jnp.arange(nb)[:, None] * Q_BLOCK + jnp.arange(kb_len)[None, :]
    kb = kp[:, key_idx].astype(jnp.float32)
    vb = vp[:, key_idx].astype(jnp.float32)
    q_pos = jnp.arange(Lp).reshape(nb, Q_BLOCK)[:, :, None]
    k_pos = (key_idx - radius)[:, None, :]
    mask = (jnp.abs(q_pos - k_pos) <= radius) & (k_pos >= 0) & (k_pos < L)
    s = jnp.einsum('nbqd,nbkd->nbqk', qb.astype(jnp.float32), kb) * (Dh ** -0.5)
    s = jnp.where(mask, s, NEG_BIG)
    m = jnp.max(s, axis=-1)
    p = jnp.exp(s - m[..., None])
    denom = jnp.sum(p, axis=-1)
    o = jnp.einsum('nbqk,nbkd->nbqd', p, vb) / denom[..., None]
    lse = m + jnp.log(denom)
    return o.reshape(N, Lp, Dh)[:, :L], lse.reshape(N, Lp)[:, :L]


def dilated_attention(q, k, v):
    B, H, S, Dh = q.shape
    outs, lses = [], []
    for window, dil in DILATED_PATTERNS:
        radius = window // (2 * dil)
        L = S // dil

        def to_sub(t):
            return t.reshape(B, H, L, dil, Dh).transpose(0, 1, 3, 2, 4).reshape(B * H * dil, L, Dh)

        o, lse = banded_attention(to_sub(q), to_sub(k), to_sub(v), radius)
        outs.append(o.reshape(B, H, dil, L, Dh).transpose(0, 1, 3, 2, 4).reshape(B, H, S, Dh))
        lses.append(lse.reshape(B, H, dil, L).transpose(0, 1, 3, 2).reshape(B, H, S))
    wts = jax.nn.softmax(jnp.stack(lses, axis=0), axis=0)
    return jnp.sum(wts[..., None] * jnp.stack(outs, axis=0), axis=0)


def hier_moe(h, wg, bg, we, be, w_gate, w_up, w_down):
    T = h.shape[0]
    hf = h.astype(jnp.float32)
    gl = hf @ wg.astype(jnp.float32) + bg
    gsel = jnp.argmax(gl, axis=-1)
    g1 = jnp.take_along_axis(jax.nn.softmax(gl, axis=-1), gsel[:, None], axis=1)[:, 0]
    el_all = jnp.einsum('td,gde->tge', hf, we.astype(jnp.float32)) + be
    el = jnp.take_along_axis(el_all, gsel[:, None, None], axis=1)[:, 0]
    topv, topi = lax.top_k(el, EXPERT_TOPK)
    ww = jax.nn.softmax(topv, axis=-1)
    within = jnp.sum(jax.nn.one_hot(topi, EXPERTS_PER_GROUP, dtype=jnp.float32) * ww[..., None], axis=1)
    combine = (g1[:, None, None]
               * jax.nn.one_hot(gsel, N_GROUPS, dtype=jnp.float32)[:, :, None]
               * within[:, None, :]).reshape(T, N_EXPERTS).astype(h.dtype)
    y = jnp.zeros_like(h)
    for e in range(N_EXPERTS):
        hid = jax.nn.silu(h @ w_gate[e]) * (h @ w_up[e])
        y = y + combine[:, e, None] * (hid @ w_down[e])
    return y


def setup_inputs(seed: int = 0) -> dict:
    key = jax.random.key(seed)
    ks = jax.random.split(key, 26)
    f32 = jnp.float32

    def nrm(k, shape, scale):
        return jax.random.normal(k, shape, f32) * scale

    return {
        "x": nrm(ks[0], (BATCH, SEQ, D_MODEL), 1.0),
        "positions": jnp.tile(jnp.arange(SEQ, dtype=jnp.int32)[None, :], (BATCH, 1)),
        "norm_mix": 1.0 + nrm(ks[1], (DEPTH, D_MODEL), 0.02),
        "w_in": nrm(ks[2], (DEPTH, D_MODEL, N_IN), D_MODEL ** -0.5),
        "mu_shift": jax.random.uniform(ks[3], (DEPTH, 2, RWKV_COLS), f32, 0.0, 0.5),
        "w0": jax.random.uniform(ks[4], (DEPTH, 2, D_RWKV), f32, -4.0, 1.0),
        "w2": nrm(ks[5], (DEPTH, 2, DECAY_LORA, D_RWKV), 0.5 * DECAY_LORA ** -0.5),
        "a0": nrm(ks[6], (DEPTH, 2, D_RWKV), 0.1),
        "a2": nrm(ks[7], (DEPTH, 2, ICLR_LORA, D_RWKV), 0.5 * ICLR_LORA ** -0.5),
        "g2": nrm(ks[8], (DEPTH, GATE_LORA, D_RWKV), GATE_LORA ** -0.5),
        "k_k": 0.85 + nrm(ks[9], (DEPTH, D_RWKV), 0.02),
        "k_a": 1.0 + nrm(ks[10], (DEPTH, D_RWKV), 0.02),
        "r_k": nrm(ks[11], (DEPTH, H_RWKV, HEAD_DIM), 0.1),
        "ln_x_w": 1.0 + nrm(ks[12], (DEPTH, D_RWKV), 0.02),
        "ln_x_b": nrm(ks[13], (DEPTH, D_RWKV), 0.02),
        "w_out": nrm(ks[14], (DEPTH, D_RWKV + D_ATTN, D_MODEL), D_MODEL ** -0.5),
        "norm_ffn": 1.0 + nrm(ks[15], (DEPTH, D_MODEL), 0.02),
        "router_group_w": nrm(ks[16], (DEPTH, D_MODEL, N_GROUPS), D_MODEL ** -0.5),
        "router_group_b": nrm(ks[17], (DEPTH, N_GROUPS), 0.01),
        "router_expert_w": nrm(ks[18], (DEPTH, N_GROUPS, D_MODEL, EXPERTS_PER_GROUP), D_MODEL ** -0.5),
        "router_expert_b": nrm(ks[19], (DEPTH, N_GROUPS, EXPERTS_PER_GROUP), 0.01),
        "w_gate": nrm(ks[20], (DEPTH, N_EXPERTS, D_MODEL, D_EXPERT), D_MODEL ** -0.5),
        "w_up": nrm(ks[21], (DEPTH, N_EXPERTS, D_MODEL, D_EXPERT), D_MODEL ** -0.5),
        "w_down": nrm(ks[22], (DEPTH, N_EXPERTS, D_EXPERT, D_MODEL), D_EXPERT ** -0.5),
        "norm_final": 1.0 + nrm(ks[23], (D_MODEL,), 0.02),
    }


def reference(x, positions, norm_mix, w_in, mu_shift, w0, w2, a0, a2, g2, k_k, k_a, r_k,
              ln_x_w, ln_x_b, w_out, norm_ffn, router_group_w, router_group_b,
              router_expert_w, router_expert_b, w_gate, w_up, w_down, norm_final):
    B, S, D = x.shape
    for l in range(DEPTH):
        h = rms_norm(x, norm_mix[l])
        z = h @ w_in[l]
        z_rwkv, z_attn = z[..., :RWKV_COLS], z[..., RWKV_COLS:]
        y_rwkv = rwkv7_mixer(z_rwkv, mu_shift[l], w0[l], w2[l], a0[l], a2[l], g2[l],
                             k_k[l], k_a[l], r_k[l], ln_x_w[l], ln_x_b[l])
        q, k, v = jnp.split(z_attn.reshape(B, S, 3 * H_ATTN, HEAD_DIM), 3, axis=2)
        q = partial_rotary(q, positions)
        k = partial_rotary(k, positions)
        y_attn = dilated_attention(q.transpose(0, 2, 1, 3), k.transpose(0, 2, 1, 3),
                                   v.transpose(0, 2, 1, 3))
        y_attn = y_attn.transpose(0, 2, 1, 3).reshape(B, S, D_ATTN).astype(x.dtype)
        x = x + jnp.concatenate([y_rwkv, y_attn], axis=-1) @ w_out[l]
        h = rms_norm(x, norm_ffn[l])
        y_moe = hier_moe(h.reshape(B * S, D), router_group_w[l], router_group_b[l],
                         router_expert_w[l], router_expert_b[l],
                         w_gate[l], w_up[l], w_down[l])
        x = x + y_moe.reshape(B, S, D)
    return rms_norm(x, norm_final)
```

```python
import numpy as np
import concourse.bass as bass
import concourse.mybir as mybir
from concourse.bass_utils import run_bass_kernel_spmd
from contextlib import ExitStack

F32 = mybir.dt.float32
BF16 = mybir.dt.bfloat16
I32 = mybir.dt.int32
AF = mybir.ActivationFunctionType
ALU = mybir.AluOpType

NCORE = 8
SEQ = 8192
D = 2048
DC = 16
TILE = 256
NT = SEQ // TILE
CH = 64
NCHK = TILE // CH
TOK_PER_CORE = SEQ // NCORE
ENGS = ("pe", "act", "dve", "pool", "sp")


class Buf:
    __slots__ = ("name", "w", "r")

    def __init__(self, name):
        self.name = name
        self.w = None
        self.r = {}


class Op:
    __slots__ = ("eng", "fns", "deps", "signal", "val", "is_dma", "semkey", "inc")

    def __init__(self, eng, fns, is_dma=False, semkey=None, inc=16):
        self.eng = eng
        self.fns = fns
        self.deps = {}
        self.signal = False
        self.val = None
        self.is_dma = is_dma
        self.semkey = semkey
        self.inc = inc


def _isap(x):
    return hasattr(x, "tensor")


class Sched:
    def __init__(self, nc):
        self.nc = nc
        self.es = ExitStack()
        self.sems = {}
        self.cnt = {}
        self.waited = {e: {} for e in ENGS}
        self.engobj = {"pe": nc.tensor, "act": nc.scalar, "dve": nc.vector,
                       "pool": nc.gpsimd, "sp": nc.sync}
        self.nops = {e: 0 for e in ENGS}
        self.reset()

    def reset(self):
        self.bufs = {}
        self.streams = {e: [] for e in ENGS}
        self.keymap = {}

    def sb(self, stack, name, shape, dtype):
        return stack.enter_context(self.nc.sbuf_tensor(name, list(shape), dtype))

    def ps(self, stack, name, shape, dtype=F32):
        return stack.enter_context(self.nc.psum_tensor(name, list(shape), dtype))

    def buf_of(self, a):
        if isinstance(a, Buf):
            return a
        if isinstance(a, str):
            name = a
        else:
            name = a.tensor.name
        b = self.bufs.get(name)
        if b is None:
            b = self.bufs[name] = Buf(name)
        return b

    def _track(self, op, ins, outs):
        for a in ins:
            b = self.buf_of(a)
            if b.w is not None and b.w is not op:
                op.deps[b.w] = True
        for a in outs:
            b = self.buf_of(a)
            if b.w is not None and b.w is not op:
                op.deps.setdefault(b.w, False)
            for r in b.r.values():
                if r is not op:
                    op.deps.setdefault(r, False)
        for a in ins:
            self.buf_of(a).r[id(op) if op.is_dma else op.eng] = op
        for a in outs:
            b = self.buf_of(a)
            b.w = op
            b.r = {}

    def emit(self, eng, meth, *args, ins=(), outs=(), **kw):
        def fn(e, meth=meth, args=args, kw=kw):
            return getattr(e, meth)(*args, **kw)
        op = Op(eng, [fn])
        self._track(op, ins, outs)
        self.streams[eng].append(op)
        return op

    def dma(self, q, pairs, semkey, ins=(), outs=(), **kw):
        fns = []
        for (o, i) in pairs:
            def fn(e, o=o, i=i, kw=kw):
                return e.dma_start(out=o, in_=i, **kw)
            fns.append(fn)
        ins = list(ins) + [i for (o, i) in pairs]
        outs = list(outs) + [o for (o, i) in pairs]
        op = Op(q, fns, is_dma=True, semkey=semkey)
        self._track(op, ins, outs)
        self.streams[q].append(op)
        return op

    def collective(self, kind, alu, in_t, out_t, semkey):
        def fn(e):
            return e.collective_compute(kind, alu, replica_groups=[list(range(NCORE))],
                                        ins=[in_t.ap().opt()], outs=[out_t.ap().opt()])
        op = Op("pool", [fn], is_dma=True, semkey=semkey, inc=1)
        self._track(op, [in_t.ap()], [out_t.ap()])
        self.streams["pool"].append(op)
        return op

    def _key(self, op):
        if not op.is_dma:
            return "e_" + op.eng
        if op.inc == 1:
            return "c_cc"
        k = self.keymap.get(op.semkey)
        if k is None:
            k = self.keymap[op.semkey] = "d_%d" % len(self.keymap)
        return k

    def _sem(self, key):
        s = self.sems.get(key)
        if s is None:
            s = self.sems[key] = self.es.enter_context(self.nc.semaphore(key))
        return s

    def flush(self, final=False):
        nc = self.nc
        for eng, s in self.streams.items():
            for op in s:
                for d, raw in op.deps.items():
                    if d.is_dma:
                        continue
                    if (not op.is_dma) and d.eng == op.eng and (op.eng == "pe" or not raw):
                        continue
                    d.signal = True
            last = None
            for op in s:
                if not op.is_dma:
                    last = op
            if last is not None:
                last.signal = True
        for eng, s in self.streams.items():
            for op in s:
                if op.is_dma:
                    k = self._key(op)
                    self.cnt[k] = self.cnt.get(k, 0) + op.inc * len(op.fns)
                    op.val = self.cnt[k]
                    self._sem(k)
                elif op.signal:
                    k = self._key(op)
                    self.cnt[k] = self.cnt.get(k, 0) + 1
                    op.val = self.cnt[k]
                    self._sem(k)
        totals = dict(self.cnt)
        with nc.Block() as block:
            def run_stream(engname, e):
                waited = self.waited[engname]
                for op in self.streams[engname]:
                    for d, raw in op.deps.items():
                        if (not d.is_dma) and (not op.is_dma) and d.eng == engname \
                                and (engname == "pe" or not raw):
                            continue
                        k = self._key(d)
                        if waited.get(k, 0) >= d.val:
                            continue
                        e.wait_ge(self.sems[k], d.val)
                        waited[k] = d.val
                    for fn in op.fns:
                        inst = fn(e)
                        if op.is_dma:
                            inst.then_inc(self.sems[self._key(op)], op.inc)
                        elif op.signal:
                            inst.then_inc(self.sems[self._key(op)], 1)
                    self.nops[engname] += len(op.fns)
                for k, v in totals.items():
                    if waited.get(k, 0) >= v:
                        continue
                    e.wait_ge(self.sems[k], v)
                    waited[k] = v

            @block.tensor
            def _(e):
                run_stream("pe", e)

            @block.scalar
            def _(e):
                run_stream("act", e)

            @block.vector
            def _(e):
                run_stream("dve", e)

            @block.gpsimd
            def _(e):
                run_stream("pool", e)

            @block.sync
            def _(e):
                run_stream("sp", e)
        self.reset()


SHIFT_GROUPS = ["r0", "r1", "k0", "k1", "v0", "v1", "wlf", "alf", "wlb", "alb", "gla", "glb"]
P1_GROUPS = ["r0", "r1", "k0", "k1", "v0", "v1", "wlf", "alf"]
P3_GROUPS = ["aq", "ak", "av"]
P2_GROUPS = ["r0", "r1", "k0", "k1", "v0", "v1", "wlb", "alb", "gla", "glb"]
GW = {"r0": 64, "r1": 64, "k0": 64, "k1": 64, "v0": 64, "v1": 64, "wlf": 64, "alf": 64,
      "wlb": 64, "alb": 64, "gla": 128, "glb": 32, "aq": 128, "ak": 128, "av": 128}


def group_offsets(groups):
    off, o = {}, 0
    for g in groups:
        off[g] = o
        o += GW[g]
    return off, o


P1_OFF, P1_N = group_offsets(P1_GROUPS)
P2_OFF, P2_N = group_offsets(P2_GROUPS)
P3_OFF, P3_N = group_offsets(P3_GROUPS)


def pt_names():
    n = []
    for g in SHIFT_GROUPS:
        n += ["mp_" + g, "mn_" + g]
    n += ["kk0", "kk1", "ka0", "ka1", "rk0", "rk1", "lnw0", "lnw1", "lnb0", "lnb1",
          "w0f0", "w0f1", "w0b0", "w0b1", "a0f0", "a0f1", "a0b0", "a0b1", "invf"]
    return n


PT_NAMES = pt_names()
PT_IDX = {n: i for i, n in enumerate(PT_NAMES)}
NPT = len(PT_NAMES)

C_IDENT = 0
C_ROTM = 128
C_TRIS = 256
C_TRII = 256 + 512
C_TRIST = 256 + 1024
C_IDR = 256 + 1536
C_ONES = 256 + 2048
C_SCAN = C_ONES + 64
C_SEL = C_SCAN + 256
C_BLK = C_SEL + 64
NCST = C_BLK + 128

NKB = 20


def make_consts():
    c = np.zeros((128, NCST), np.float32)
    c[:, C_IDENT:C_IDENT + 128] = np.eye(128, dtype=np.float32)
    rot = np.zeros((128, 128), np.float32)
    for h in range(2):
        for i in range(8):
            rot[h * 64 + 8 + i, h * 64 + i] = -1.0
            rot[h * 64 + i, h * 64 + 8 + i] = 1.0
    c[:, C_ROTM:C_ROTM + 128] = rot
    s = np.arange(64)[:, None]
    t = np.arange(64)[None, :]
    tri_s = (s < t).astype(np.float32)
    tri_i = (s <= t).astype(np.float32)
    c[:64, C_TRIS:C_TRIS + 512] = np.tile(tri_s, (1, 8))
    c[:64, C_TRII:C_TRII + 512] = np.tile(tri_i, (1, 8))
    c[:64, C_TRIST:C_TRIST + 512] = np.tile(tri_s.T, (1, 8))
    c[:64, C_IDR:C_IDR + 512] = np.tile(np.eye(64, dtype=np.float32), (1, 8))
    c[:, C_ONES:C_ONES + 64] = 1.0
    sm = np.ones((256,), np.float32)
    sm[::64] = 0.0
    c[:, C_SCAN:C_SCAN + 256] = sm[None, :]
    c[64, C_SEL:C_SEL + 64] = 1.0
    return c


def make_attn_mask():
    k = np.arange(128)[:, None, None]
    off = np.arange(NKB)[None, :, None]
    q = np.arange(512)[None, None, :]
    d = (off - 8) * 128 + k - q
    ad = np.abs(d)
    m = (ad <= 64).astype(np.float32) + ((d % 4 == 0) & (ad <= 256)).astype(np.float32) \
        + ((d % 16 == 0) & (ad <= 1024)).astype(np.float32)
    return np.ascontiguousarray(m.astype(np.float32))


def prep_inputs(inputs):
    x = np.ascontiguousarray(np.asarray(inputs["x"])[0])
    pos = np.ascontiguousarray(np.asarray(inputs["positions"])[0].astype(np.int32))
    w_in = np.asarray(inputs["w_in"])[0]
    mu = np.asarray(inputs["mu_shift"])[0]
    w0 = np.asarray(inputs["w0"])[0]
    w2 = np.asarray(inputs["w2"])[0]
    a0 = np.asarray(inputs["a0"])[0]
    a2 = np.asarray(inputs["a2"])[0]
    g2 = np.asarray(inputs["g2"])[0]
    k_k = np.asarray(inputs["k_k"])[0]
    k_a = np.asarray(inputs["k_a"])[0]
    r_k = np.asarray(inputs["r_k"])[0]
    ln_w = np.asarray(inputs["ln_x_w"])[0]
    ln_b = np.asarray(inputs["ln_x_b"])[0]
    w_out = np.ascontiguousarray(np.asarray(inputs["w_out"])[0])
    gmix = np.ascontiguousarray(np.asarray(inputs["norm_mix"])[0].reshape(DC, 128).T)
    gffn = np.ascontiguousarray(np.asarray(inputs["norm_ffn"])[0].reshape(DC, 128).T)
    gfin = np.ascontiguousarray(np.asarray(inputs["norm_final"]).reshape(D))
    rw = np.asarray(inputs["router_group_w"])[0]
    rew = np.asarray(inputs["router_expert_w"])[0]
    wr = np.ascontiguousarray(np.concatenate([rw] + [rew[g] for g in range(4)], axis=1))
    br = np.ascontiguousarray(np.concatenate([np.asarray(inputs["router_group_b"])[0].reshape(-1),
                                              np.asarray(inputs["router_expert_b"])[0].reshape(-1)]))
    wg = np.asarray(inputs["w_gate"])[0]
    wu = np.asarray(inputs["w_up"])[0]
    wd = np.asarray(inputs["w_down"])[0]
    consts = make_consts()
    amask = make_attn_mask()
    inv_freq = (500000.0 ** (-np.arange(8, dtype=np.float32) * 2.0 / 16.0)).astype(np.float32)
    maps = []
    for c in range(NCORE):
        h = (2 * c, 2 * c + 1)

        def hcol(base, hh):
            return np.arange(base + hh * 64, base + hh * 64 + 64)
        cols = {
            "r0": hcol(0, h[0]), "r1": hcol(0, h[1]), "k0": hcol(1024, h[0]), "k1": hcol(1024, h[1]),
            "v0": hcol(2048, h[0]), "v1": hcol(2048, h[1]),
            "wlf": 3072 + np.arange(64), "wlb": 3136 + np.arange(64),
            "alf": 3200 + np.arange(64), "alb": 3264 + np.arange(64),
            "gla": 3328 + np.arange(128), "glb": 3456 + np.arange(32),
            "aq": np.concatenate([hcol(3488, h[0]), hcol(3488, h[1])]),
            "ak": np.concatenate([hcol(3488 + 1024, h[0]), hcol(3488 + 1024, h[1])]),
            "av": np.concatenate([hcol(3488 + 2048, h[0]), hcol(3488 + 2048, h[1])]),
        }
        w1 = np.ascontiguousarray(w_in[:, np.concatenate([cols[g] for g in P1_GROUPS])])
        w2p = np.ascontiguousarray(w_in[:, np.concatenate([cols[g] for g in P2_GROUPS])])
        w3 = np.ascontiguousarray(w_in[:, np.concatenate([cols[g] for g in P3_GROUPS])])
        pt = np.zeros((128, NPT), np.float32)

        def put(name, vec):
            pt[:len(vec), PT_IDX[name]] = vec
        for g in SHIFT_GROUPS:
            put("mp_" + g, mu[0][cols[g]])
            put("mn_" + g, mu[1][cols[g]])
        for i in range(2):
            ch = np.arange(h[i] * 64, h[i] * 64 + 64)
            put("kk%d" % i, k_k[ch])
            put("ka%d" % i, k_a[ch])
            put("rk%d" % i, r_k[h[i]])
            put("lnw%d" % i, ln_w[ch])
            put("lnb%d" % i, ln_b[ch])
            put("w0f%d" % i, w0[0][ch])
            put("w0b%d" % i, w0[1][ch])
            put("a0f%d" % i, a0[0][ch])
            put("a0b%d" % i, a0[1][ch])
        invf = np.zeros((128,), np.float32)
        for hh in range(2):
            invf[hh * 64:hh * 64 + 8] = inv_freq
            invf[hh * 64 + 8:hh * 64 + 16] = inv_freq
        put("invf", invf)
        chs = np.concatenate([np.arange(h[0] * 64, h[0] * 64 + 64), np.arange(h[1] * 64, h[1] * 64 + 64)])
        lw = np.zeros((64, 8, 64), np.float32)
        for d_ in range(2):
            for i in range(2):
                ch = np.arange(h[i] * 64, h[i] * 64 + 64)
                lw[:, 0 * 4 + d_ * 2 + i, :] = w2[d_][:, ch]
                lw[:, 1 * 4 + d_ * 2 + i, :] = a2[d_][:, ch]
        oh = np.zeros((128, 8), np.float32)
        oh[:, c] = 1.0
        m = {
            "oh": oh, "xs": np.ascontiguousarray(x[c * TOK_PER_CORE:(c + 1) * TOK_PER_CORE]),
            "x": x, "pos": pos, "w1": w1, "w2p": w2p, "w3": w3, "pt": pt, "cst": consts, "amask": amask,
            "gmix": gmix, "gffn": gffn, "gfin": gfin,
            "lw": np.ascontiguousarray(lw.reshape(64, 512)),
            "g2a": np.ascontiguousarray(g2[0:128][:, chs]), "g2b": np.ascontiguousarray(g2[128:160][:, chs]),
            "w_out": w_out, "wr": wr, "br": br,
            "wg": np.ascontiguousarray(wg[4 * c:4 * c + 4]), "wu": np.ascontiguousarray(wu[4 * c:4 * c + 4]),
            "wd": np.ascontiguousarray(wd[4 * c:4 * c + 4]),
        }
        maps.append(m)
    return maps


class KB:
    def __init__(self, nc):
        self.nc = nc
        self.S = Sched(nc)
        self.rr = 0

    def act(self, out, in_, func, bias=None, scale=None, accum=None):
        kw, ins, outs = {}, [in_], [out]
        if bias is not None:
            kw["bias"] = bias
            if _isap(bias):
                ins.append(bias)
        if scale is not None:
            kw["scale"] = scale
            if _isap(scale):
                ins.append(scale)
        if accum is not None:
            kw["accum_out"] = accum
            outs.append(accum)
        return self.S.emit("act", "activation", out, in_, func, ins=ins, outs=outs, **kw)

    def tt(self, eng, out, in0, in1, op):
        return self.S.emit(eng, "tensor_tensor", out, in0, in1, op, ins=[in0, in1], outs=[out])

    def ts(self, eng, out, in0, s1, s2, op0, op1=None):
        ins = [in0] + [s for s in (s1, s2) if _isap(s)]
        if op1 is None:
            return self.S.emit(eng, "tensor_scalar", out, in0, s1, None, op0, ins=ins, outs=[out])
        return self.S.emit(eng, "tensor_scalar", out, in0, s1, s2, op0, op1, ins=ins, outs=[out])

    def stt(self, out, in0, scalar, in1, op0, op1):
        ins = [in0, in1] + ([scalar] if _isap(scalar) else [])
        return self.S.emit("dve", "scalar_tensor_tensor", out, in0, scalar, in1, op0, op1, ins=ins, outs=[out])

    def cp(self, eng, out, in_):
        if eng == "act":
            return self.act(out, in_, AF.Copy)
        return self.S.emit(eng, "tensor_copy", out, in_, ins=[in_], outs=[out])

    def memset(self, eng, out, val):
        return self.S.emit(eng, "memset", out, val, outs=[out])

    def recip(self, out, in_):
        return self.S.emit("dve", "reciprocal", out, in_, ins=[in_], outs=[out])

    def mm(self, out, lhsT, rhs, start=True, stop=True, extra_out=None):
        ins = [lhsT, rhs] + ([] if start else [out])
        outs = [out] if extra_out is None else [extra_out]
        return self.S.emit("pe", "matmul", out, lhsT, rhs, start=start, stop=stop, ins=ins, outs=outs)

    def tr(self, out, in_, ident):
        return self.S.emit("pe", "transpose", out, in_, ident, ins=[in_, ident], outs=[out])

    def scan(self, out, d0, d1, init, op0, op1):
        return self.S.emit("dve", "tensor_tensor_scan", out, d0, d1, init, op0, op1, ins=[d0, d1], outs=[out])

    def rsqrt(self, out, in_, eps):
        self.ts("dve", out, in_, eps, None, ALU.add)
        self.act(out, out, AF.Sqrt)
        self.recip(out, out)


def v3(ap, inner):
    return ap.rearrange("p (a b) -> p a b", b=inner)


def build(upto=99, debug=False, launch=None):
    nc = bass.Bass("TRN2", target_bir_lowering=False)
    K = KB(nc)
    S = K.S
    declared = []

    LUSE = {"x": (1,), "pos": (1,), "w1": (1,), "w2p": (1,), "w3": (1,), "amask": (1,), "lw": (1,), "g2a": (1,), "g2b": (1,),
            "gmix": (1,), "pt": (1,), "cst": (1, 2), "gffn": (2, 3), "gfin": (4,), "w_out": (2,), "oh": (), "xs": (2,),
            "wr": (2,), "br": (2,), "wg": (3,), "wu": (3,), "wd": (3,)}

    def dt_in(name, shape, dt=F32, need=0):
        if upto < need:
            return None
        if launch is not None and launch not in LUSE[name]:
            return None
        declared.append(name)
        return nc.dram_tensor(name, list(shape), dt, kind="ExternalInput").ap()
    x_d = dt_in("x", [SEQ, D])
    pos_d = dt_in("pos", [SEQ], I32)
    w1_d = dt_in("w1", [D, P1_N])
    w2p_d = dt_in("w2p", [D, P2_N], need=2)
    w3_d = dt_in("w3", [D, P3_N], need=3)
    pt_d = dt_in("pt", [128, NPT])
    cst_d = dt_in("cst", [128, NCST])
    amask_d = dt_in("amask", [128, NKB, 512], need=3)
    gmix_d = dt_in("gmix", [128, DC])
    gffn_d = dt_in("gffn", [128, DC], need=4)
    gfin_d = dt_in("gfin", [D], need=6)
    lw_d = dt_in("lw", [64, 512])
    g2a_d = dt_in("g2a", [128, 128])
    g2b_d = dt_in("g2b", [32, 128])
    wout_d = dt_in("w_out", [D, D], need=4)
    oh_d = dt_in("oh", [128, 8], need=4)
    xs_d = dt_in("xs", [TOK_PER_CORE, D], need=4)
    wr_d = dt_in("wr", [D, 36], need=4)
    br_d = dt_in("br", [36], need=4)
    wg_d = dt_in("wg", [4, D, 1024], need=5)
    wu_d = dt_in("wu", [4, D, 1024], need=5)
    wd_d = dt_in("wd", [4, 1024, D], need=5)
    out_d = None
    if launch in (None, 4):
        out_d = nc.dram_tensor("out", [TOK_PER_CORE, D], F32, kind="ExternalOutput").ap()
    ext_in = lambda name, shape, dt=F32: (declared.append(name), nc.dram_tensor(name, list(shape), dt, kind="ExternalInput").ap())[1]
    ext_out = lambda name, shape, dt=F32: nc.dram_tensor(name, list(shape), dt, kind="ExternalOutput").ap()
    L = launch
    dbg = {}
    if debug:
        dbg["yb"] = nc.dram_tensor("dbg_yb", [256, SEQ], BF16, kind="ExternalOutput").ap()
        dbg["yf"] = nc.dram_tensor("dbg_yf", [64, 2, SEQ], F32, kind="ExternalOutput").ap()
        dbg["x2"] = nc.dram_tensor("dbg_x2", [TOK_PER_CORE, D], F32, kind="ExternalOutput").ap()
        dbg["comb"] = nc.dram_tensor("dbg_comb", [TOK_PER_CORE, 32], F32, kind="ExternalOutput").ap()
    yf_t = nc.dram_tensor("yf_scr", [64, 2, SEQ], F32)
    ybounce_t = nc.dram_tensor("ybounce", [256, SEQ], BF16)
    yg_t = nc.dram_tensor("ygath", [NCORE * 256, SEQ], BF16)
    yf_d, ybounce_d, yg_d = yf_t.ap(), ybounce_t.ap(), yg_t.ap()
    if L == 1:
        ybounce_d = ext_out("ybo", [256, SEQ], BF16)
    x2s_t = nc.dram_tensor("x2_scr", [TOK_PER_CORE, D], F32)
    hb_t = nc.dram_tensor("h2bounce", [D, TOK_PER_CORE], BF16)
    hg_t = nc.dram_tensor("h2gath", [NCORE * D, TOK_PER_CORE], BF16)
    cb_t = nc.dram_tensor("cbounce", [TOK_PER_CORE, 32], F32)
    cg_t = nc.dram_tensor("cgath", [SEQ, 32], F32)
    wg16_t = nc.dram_tensor("wg16", [4, 128, DC, 1024], BF16)
    wu16_t = nc.dram_tensor("wu16", [4, 128, DC, 1024], BF16)
    wd16_t = nc.dram_tensor("wd16", [4, 128, 8, D], BF16)
    yp_t = nc.dram_tensor("ypart", [SEQ, D], F32)
    ym_t = nc.dram_tensor("ymoe", [TOK_PER_CORE, D], F32)
    x2s_d, hb_d, hg_d, cb_d, cg_d = x2s_t.ap(), hb_t.ap(), hg_t.ap(), cb_t.ap(), cg_t.ap()
    wg16_d, wu16_d, wd16_d, yp_d, ym_d = wg16_t.ap(), wu16_t.ap(), wd16_t.ap(), yp_t.ap(), ym_t.ap()
    yt_in = cw_in = ypi_d = None
    if L == 2:
        yt_in = ext_in("yt", [D, TOK_PER_CORE], BF16)
        x2s_d = ext_out("x2o", [TOK_PER_CORE, D])
        hb_d = ext_out("hbo", [D, TOK_PER_CORE], BF16)
        cb_d = ext_out("cbo", [TOK_PER_CORE, 32])
    if L == 3:
        hg_d = ext_in("hg", [NCORE * D, TOK_PER_CORE], BF16)
        cw_in = ext_in("cwi", [SEQ, 4])
        yp_d = ext_out("ypo", [SEQ, D])
    if L == 4:
        x2s_d = ext_in("x2i", [TOK_PER_CORE, D])
        ypi_d = ext_in("ypi", [NCORE, TOK_PER_CORE, D])

    st = ExitStack()
    sb = lambda name, shape, dt=F32, stack=None: S.sb(stack or st, name, shape, dt)

    cst = sb("cst_sb", [128, NCST])
    PT = sb("PT", [128, NPT])
    PD = sb("PD", [128, 16])
    identb = sb("identb", [128, 128], BF16)
    rotmb = sb("rotmb", [128, 128], BF16)
    trisb = sb("trisb", [64, 512], BF16)
    triib = sb("triib", [64, 512], BF16)
    tristb = sb("tristb", [64, 512], BF16)
    idrb = sb("idrb", [64, 512], BF16)
    onesb = sb("onesb", [64, 64], BF16)
    meanm = sb("meanm", [64, 64])
    lwb = sb("lwb", [64, 512], BF16)
    g2ab = sb("g2ab", [128, 128], BF16)
    g2bb = sb("g2bb", [32, 128], BF16)
    gmix = sb("gmix_sb", [128, DC])
    with ExitStack() as p0:
      if L in (None, 1, 2):
          lwf = sb("lwf", [64, 512], F32, p0)
          g2af = sb("g2af", [128, 128], F32, p0)
          g2bf = sb("g2bf", [32, 128], F32, p0)
          S.dma("sp", [(cst[:], cst_d)], "c0")
          if L != 2:
              S.dma("sp", [(PT[:], pt_d)], "c1")
              S.dma("sp", [(lwf[:], lw_d), (g2af[:], g2a_d), (g2bf[:], g2b_d), (gmix[:], gmix_d)], "c2")
          else:
              for t_ in (PT, lwf, g2af, g2bf, gmix):
                  K.memset("pool", t_[:], 0.0)
          K.cp("dve", identb[:], cst[:, C_IDENT:C_IDENT + 128])
          K.cp("dve", rotmb[:], cst[:, C_ROTM:C_ROTM + 128])
          K.cp("dve", trisb[:], cst[0:64, C_TRIS:C_TRIS + 512])
          K.cp("dve", triib[:], cst[0:64, C_TRII:C_TRII + 512])
          K.cp("pool", tristb[:], cst[0:64, C_TRIST:C_TRIST + 512])
          K.cp("pool", idrb[:], cst[0:64, C_IDR:C_IDR + 512])
          K.cp("pool", onesb[:], cst[0:64, C_ONES:C_ONES + 64])
          K.ts("pool", meanm[:], cst[0:64, C_ONES:C_ONES + 64], 1.0 / 64.0, None, ALU.mult)
          K.cp("dve", lwb[:], lwf[:])
          K.cp("dve", g2ab[:], g2af[:])
          K.cp("dve", g2bb[:], g2bf[:])
          for gi, g in enumerate(SHIFT_GROUPS):
              K.tt("dve", PD[:, gi:gi + 1], PT[:, PT_IDX["mp_" + g]:PT_IDX["mp_" + g] + 1],
                   PT[:, PT_IDX["mn_" + g]:PT_IDX["mn_" + g] + 1], ALU.add)
          K.ts("dve", PD[:, 0:12], PD[:, 0:12], -1.0, 1.0, ALU.mult, ALU.add)
          K.ts("dve", PD[:, 12:14], PT[:, PT_IDX["ka0"]:PT_IDX["ka0"] + 2], -1.0, 1.0, ALU.mult, ALU.add)
          S.flush()

    def ptc(name, rows=64):
        i = PT_IDX[name]
        return PT[0:rows, i:i + 1]

    def c0c(g):
        i = SHIFT_GROUPS.index(g)
        return PD[0:GW[g], i:i + 1]

    onesf = cst[0:64, C_ONES:C_ONES + 64]
    scanm = cst[0:64, C_SCAN:C_SCAN + 256]
    sel65 = cst[0:65, C_SEL:C_SEL + 64]

    att = ExitStack()
    AT = {}

    def alloc_att():
        AT["qT"] = sb("qT", [128, SEQ], BF16, att)
        AT["kT"] = sb("kT", [128, SEQ], BF16, att)
        AT["Vaug"] = sb("Vaug", [128, 64, 2, 65], BF16, att)

    def run_pass(pidx):
        rev = pidx == 1
        amode = pidx == 2
        rw = not amode
        goff = (P1_OFF, P2_OFF, P3_OFF)[pidx]
        ncols = (P1_N, P2_N, P3_N)[pidx]
        wsrc = (w1_d, w2p_d, w3_d)[pidx]
        dname = "b" if rev else "f"
        if amode:
            qT, kT, Vaug = AT["qT"], AT["kT"], AT["Vaug"]
        with ExitStack() as ps_:
            T = lambda name, shape, dt=F32: S.sb(ps_, "p%d_%s" % (pidx, name), shape, dt)
            wbf = T("wbf", [128, DC, ncols], BF16)
            wst = [T("wst%d" % i, [128, ncols]) for i in range(2)]
            xt = [T("xt%d" % i, [128, D]) for i in range(2)]
            xsq = T("xsq", [128, D], BF16)
            ssq = [T("ssq%d" % i, [128, 1]) for i in range(2)]
            rsx = [T("rsx%d" % i, [128, 1]) for i in range(2)]
            xn = [T("xn%d" % i, [128, D], BF16) for i in range(2)]
            hT = [T("hT%d" % i, [128, DC, TILE + 2], BF16) for i in range(3)]
            TMP1 = T("TMP1", [128, TILE])
            TMP2 = T("TMP2", [128, TILE])
            if rw:
              Rr = T("Rr", [64, 2, TILE])
              Kr = T("Kr", [64, 2, TILE])
              Vr = T("Vr", [64, 2, TILE])
              WLT = T("WLT", [64, TILE], BF16)
              ALB = T("ALB", [64, TILE], BF16)
              SW = T("SW", [64, 2, TILE])
              A_ = T("A_", [64, 2, TILE])
              LD = T("LD", [64, 2, TILE])
              KK = T("KK", [64, 2, TILE])
              KK2 = T("KK2", [64, 2, TILE], BF16)
              RIN = T("RIN", [64, 2, TILE])
              KKN = T("KKN", [64, 2, TILE])
              AN = T("AN", [64, 2, TILE])
              T1 = T("T1", [64, 2, TILE])
              KD = T("KD", [64, 2, TILE])
              BV = T("BV", [64, 2, TILE])
              CUM = T("CUM", [64, 2, TILE])
              EP = T("EP", [64, 2, TILE])
              EM = T("EM", [64, 2, TILE])
              DIF = T("DIF", [64, 2, TILE])
              EPP = T("EPP", [64, 2, TILE])
              EH = T("EH", [64, 2, TILE])
              ATRT = T("ATRT", [64, 2, NCHK, 128], BF16)
              BT = T("BT", [64, 2, TILE], BF16)
              KT = T("KT", [64, 2, TILE], BF16)
              BH = T("BH", [64, 2, TILE], BF16)
              KH = T("KH", [64, 2, TILE], BF16)
              VB = T("VB", [64, 2, TILE], BF16)
              VT = T("VT", [64, 8, 64], BF16)
              BHT = T("BHT", [64, 8, 64], BF16)
              KHT = T("KHT", [64, 8, 64], BF16)
              MAB = T("MAB", [64, 8, 64], BF16)
              MABT = T("MABT", [64, 8, 64], BF16)
              MKA = T("MKA", [64, 8, 64], BF16)
              NBR = T("NBR", [64, 8, 64], BF16)
              NKR = T("NKR", [64, 8, 64], BF16)
              Pb = [T("Pb%d" % i, [64, 8, 64], BF16) for i in range(2)]
              PTb = [T("PTb%d" % i, [64, 8, 64], BF16) for i in range(2)]
              Xb = [T("Xb%d" % i, [64, 8, 64], BF16) for i in range(2)]
              W2 = T("W2", [64, 8, 64])
              WS = T("WS", [64, 2, 64], BF16)
              UT16 = T("UT16", [64, 2, 64], BF16)
              ST32 = T("ST32", [64, 2, 64])
              ST16 = T("ST16", [64, 2, 64], BF16)
              YS = T("YS", [64, 2, TILE])
            if rev:
                SGa = T("SGa", [128, TILE], BF16)
                SGb = T("SGb", [32, TILE], BF16)
                YF = T("YF", [64, 2, TILE])
                YC = T("YC", [64, 2, TILE])
                SQ = T("SQ", [64, 2, TILE])
                RS = T("RS", [64, 2, TILE])
                YL = T("YL", [64, 2, TILE])
                RK = T("RK", [64, 2, TILE])
                BON = T("BON", [64, 2, TILE])
                OUTB = T("OUTB", [64, 2, TILE], BF16)
            if amode:
                posi = T("posi", [128, TILE], I32)
                posf = T("posf", [128, TILE])
                ANG = T("ANG", [128, TILE])
                RY = T("RY", [128, TILE])
                KQI = T("KQI", [128, TILE], I32)
                KQF = T("KQF", [128, TILE])
                G1 = T("G1", [128, TILE])
                SINT = T("SINT", [128, TILE])
                COST = T("COST", [128, TILE])
                QF = T("QF", [128, TILE])
                QB = T("QB", [128, TILE], BF16)
                QT1 = T("QT1", [128, TILE])
                QT2 = T("QT2", [128, TILE])
                VBa = T("VBa", [128, TILE], BF16)
            ptr = S.ps(ps_, "p%d_ptr" % pidx, [128, 8, 128], BF16)
            pz = [S.ps(ps_, "p%d_pz%d" % (pidx, i), [128, 512]) for i in range(2)]
            pr = [S.ps(ps_, "p%d_pr%d" % (pidx, i), [128, 512]) for i in range(3)]
            pseq = S.ps(ps_, "p%d_pseq" % pidx, [64, 512])
            pY = S.ps(ps_, "p%d_pY" % pidx, [64, 2, TILE])
            bW1, bUT, bSN = Buf("pseq_w1"), Buf("pseq_ut"), Buf("pseq_sn")
            state = {"bank": 0, "pz": 0}

            def bank():
                b = pr[state["bank"] % 3]
                state["bank"] += 1
                return b

            def pzbank():
                b = pz[state["pz"] % 2]
                state["pz"] += 1
                return b

            for dc in range(DC):
                ws = wst[dc % 2]
                S.dma("sp", [(ws[:], wsrc[dc * 128:(dc + 1) * 128, :])], "w%d" % (dc % 2))
                K.ts("dve" if dc % 2 == 0 else "pool", wbf[:, dc, :], ws[:], gmix[:, dc:dc + 1], None, ALU.mult)
            if rw:
                K.memset("dve", ST32[:], 0.0)
                K.memset("dve", ST16[:], 0.0)
            if amode:
                K.memset("pool", Vaug[:, :, :, 64:65], 1.0)

            def prep_tile(i):
                slot = i % 3
                for blk in range(2):
                    tb = 2 * i + blk
                    xs = xt[blk]
                    S.dma("sp", [(xs[:], x_d[tb * 128:(tb + 1) * 128, :])], "x%d" % blk)
                    K.act(xsq[:], xs[:], AF.Square, accum=ssq[blk][:])
                    K.ts("dve", rsx[blk][:], ssq[blk][:], 1.0 / D, 1e-6, ALU.mult, ALU.add)
                    K.act(rsx[blk][:], rsx[blk][:], AF.Sqrt)
                    K.recip(rsx[blk][:], rsx[blk][:])
                    K.ts("pool" if blk == 0 else "dve", xn[blk][:], xs[:], rsx[blk][:], None, ALU.mult)
                    for half in range(2):
                        for j in range(8):
                            dc = half * 8 + j
                            K.tr(ptr[:, j, :], xn[blk][:, dc * 128:(dc + 1) * 128], identb[:])
                        dst = hT[slot][:, half * 8:(half + 1) * 8, 1 + blk * 128:1 + blk * 128 + 128]
                        if half == 0:
                            K.cp("act", dst, ptr[:])
                        else:
                            K.cp("dve", dst, ptr[:])

            def shift(g, pzb, dst, eng2="dve", final_func=None):
                M = GW[g]
                mp = PT[0:M, PT_IDX["mp_" + g]:PT_IDX["mp_" + g] + 1]
                mn = PT[0:M, PT_IDX["mn_" + g]:PT_IDX["mn_" + g] + 1]
                K.act(TMP1[0:M, :], pzb[0:M, 1:257], AF.Copy, scale=c0c(g))
                K.stt(TMP2[0:M, :], pzb[0:M, 0:256], mp, TMP1[0:M, :], ALU.mult, ALU.add)
                if final_func is None:
                    K.stt(dst, pzb[0:M, 2:258], mn, TMP2[0:M, :], ALU.mult, ALU.add)
                else:
                    K.stt(TMP1[0:M, :], pzb[0:M, 2:258], mn, TMP2[0:M, :], ALU.mult, ALU.add)
                    K.act(dst, TMP1[0:M, :], final_func)

            def R_(ap):
                if not rev:
                    return ap
                idx = (slice(None),) * (len(ap.shape) - 1) + (slice(None, None, -1),)
                return ap[idx]

            def process_tile(i):
                slot = i % 3
                t0 = i * TILE
                h = hT[slot]
                if i > 0:
                    K.cp("pool", h[:, :, 0:1], hT[(i - 1) % 3][:, :, TILE:TILE + 1])
                else:
                    K.memset("pool", h[:, :, 0:1], 0.0)
                if i < NT - 1:
                    K.cp("pool", h[:, :, TILE + 1:TILE + 2], hT[(i + 1) % 3][:, :, 1:2])
                else:
                    K.memset("pool", h[:, :, TILE + 1:TILE + 2], 0.0)
                if rev:
                    S.dma("sp", [(YF[:], yf_d[:, :, t0:t0 + TILE])], "yfl")

                def inproj(g):
                    M = GW[g]
                    pzb = pzbank()
                    o = goff[g]
                    for dc in range(DC):
                        K.mm(pzb[0:M, 0:TILE + 2], wbf[:, dc, o:o + M], h[:, dc, :], start=(dc == 0), stop=(dc == DC - 1))
                    return pzb

                if rw:
                    for hh in range(2):
                        shift("r%d" % hh, inproj("r%d" % hh), R_(Rr[:, hh, :]))
                        shift("k%d" % hh, inproj("k%d" % hh), R_(Kr[:, hh, :]))
                        shift("v%d" % hh, inproj("v%d" % hh), R_(Vr[:, hh, :]))
                    shift("wl" + dname, inproj("wl" + dname), R_(WLT[:]), final_func=AF.Tanh)
                    shift("al" + dname, inproj("al" + dname), R_(ALB[:]))
                if rev:
                    shift("gla", inproj("gla"), R_(SGa[:]), final_func=AF.Sigmoid)
                    shift("glb", inproj("glb"), R_(SGb[:]), final_func=AF.Sigmoid)
                if amode:
                    S.dma("sp", [(posi[:], pos_d[t0:t0 + TILE].partition_broadcast(128))], "pos")
                    K.cp("pool", posf[:], posi[:])
                    K.ts("pool", ANG[:], posf[:], ptc("invf", 128), None, ALU.mult)
                    for (tab, shiftv) in ((SINT, 0.0), (COST, float(np.pi / 2))):
                        K.ts("pool", RY[:], ANG[:], shiftv, None, ALU.add)
                        K.ts("pool", KQI[:], RY[:], float(1.0 / (2 * np.pi)), None, ALU.mult)
                        K.cp("pool", KQF[:], KQI[:])
                        K.ts("pool", KQF[:], KQF[:], float(-2 * np.pi), None, ALU.mult)
                        K.tt("pool", RY[:], RY[:], KQF[:], ALU.add)
                        K.ts("pool", G1[:], RY[:], float(np.pi), float(-2 * np.pi), ALU.is_gt, ALU.mult)
                        K.tt("pool", RY[:], RY[:], G1[:], ALU.add)
                        K.ts("pool", G1[:], RY[:], float(-np.pi), float(2 * np.pi), ALU.is_lt, ALU.mult)
                        K.tt("pool", RY[:], RY[:], G1[:], ALU.add)
                        K.ts("pool", RY[:], RY[:], float(np.pi), float(-np.pi), ALU.min, ALU.max)
                        K.act(tab[:], RY[:], AF.Sin)
                    for (g, dstT) in (("aq", qT), ("ak", kT)):
                        pzb = inproj(g)
                        K.act(QF[:], pzb[:, 1:TILE + 1], AF.Copy)
                        K.cp("dve", QB[:], pzb[:, 1:TILE + 1])
                        pb = bank()
                        K.mm(pb[:, 0:TILE], rotmb[:], QB[:])
                        K.tt("pool", QT1[:], QF[:], COST[:], ALU.mult)
                        K.tt("dve", QT2[:], pb[:, 0:TILE], SINT[:], ALU.mult)
                        K.tt("pool", dstT[:, t0:t0 + TILE], QT1[:], QT2[:], ALU.add)
                    pzb = inproj("av")
                    K.act(VBa[:], pzb[:, 1:TILE + 1], AF.Copy)
                    pb = bank()
                    pbv = pb[:, 0:128].bitcast(BF16).rearrange("p (b c) -> p b c", c=128)
                    for blk in range(2):
                        K.tr(pbv[:, blk, :], VBa[:, blk * 128:(blk + 1) * 128], identb[:])
                    K.cp("dve", Vaug[:, 2 * i:2 * i + 2, :, 0:64],
                         pbv.rearrange("p b (h d) -> p b h d", d=64))
                    return

                d_ = 1 if rev else 0
                pw_, pa_ = bank(), bank()
                pwv, pav = v3(pw_[0:64, :], TILE), v3(pa_[0:64, :], TILE)
                for hh in range(2):
                    K.mm(pwv[:, hh, :], lwb[:, (0 * 4 + d_ * 2 + hh) * 64:(0 * 4 + d_ * 2 + hh) * 64 + 64], WLT[:])
                    K.mm(pav[:, hh, :], lwb[:, (1 * 4 + d_ * 2 + hh) * 64:(1 * 4 + d_ * 2 + hh) * 64 + 64], ALB[:])
                for hh in range(2):
                    K.act(SW[:, hh, :], pwv[:, hh, :], AF.Sigmoid, bias=ptc("w0%s%d" % (dname, hh)))
                    K.act(A_[:, hh, :], pav[:, hh, :], AF.Sigmoid, bias=ptc("a0%s%d" % (dname, hh)))
                K.ts("dve", LD[:], SW[:], -0.6065306597126334, None, ALU.mult)
                for hh in range(2):
                    K.ts("dve", KK[:, hh, :], Kr[:, hh, :], ptc("kk%d" % hh), None, ALU.mult)
                K.tt("pool", KK2[:], KK[:], KK[:], ALU.mult)
                pq_ = bank()
                pqv = v3(pq_[0:64, :], TILE)
                for hh in range(2):
                    K.mm(pqv[:, hh, :], onesb[:], KK2[:, hh, :])
                K.rsqrt(RIN[:], pqv, 1e-12)
                K.tt("dve", KKN[:], KK[:], RIN[:], ALU.mult)
                K.ts("pool", AN[:], KKN[:], -1.0, None, ALU.mult)
                for hh in range(2):
                    K.ts("dve", T1[:, hh, :], A_[:, hh, :], ptc("ka%d" % hh), PD[0:64, 12 + hh:13 + hh], ALU.mult, ALU.add)
                K.tt("pool", KD[:], Kr[:], T1[:], ALU.mult)
                K.tt("pool", BV[:], KKN[:], A_[:], ALU.mult)
                for hh in range(2):
                    K.scan(CUM[:, hh, :], scanm, LD[:, hh, :], 0.0, ALU.mult, ALU.add)
                K.act(EP[:], CUM[:], AF.Exp)
                K.act(EM[:], CUM[:], AF.Exp, scale=-1.0)
                K.tt("pool", DIF[:], CUM[:], LD[:], ALU.subtract)
                K.act(EPP[:], DIF[:], AF.Exp)
                c4 = lambda ap: ap.rearrange("p h (c t) -> p h c t", t=CH)
                K.tt("dve", ATRT[:, :, :, 0:64], c4(AN[:]), c4(EPP[:]), ALU.mult)
                K.tt("pool", ATRT[:, :, :, 64:128], c4(Rr[:]), c4(EP[:]), ALU.mult)
                K.tt("dve", BT[:], BV[:], EM[:], ALU.mult)
                K.tt("pool", KT[:], KD[:], EM[:], ALU.mult)
                for hh in range(2):
                    for c in range(NCHK):
                        K.ts("dve" if c % 2 == 0 else "pool", DIF[:, hh, c * CH:(c + 1) * CH], CUM[:, hh, c * CH:(c + 1) * CH],
                             -1.0, CUM[:, hh, c * CH + CH - 1:c * CH + CH], ALU.mult, ALU.add)
                K.act(EH[:], DIF[:], AF.Exp)
                K.tt("dve", BH[:], BV[:], EH[:], ALU.mult)
                K.tt("pool", KH[:], KD[:], EH[:], ALU.mult)
                K.cp("pool", VB[:], Vr[:])
                for (src, dstT_) in ((VB, VT), (BH, BHT), (KH, KHT)):
                    pb = bank()
                    pbv = pb[0:64, 0:256].bitcast(BF16).rearrange("p (a b) -> p a b", b=64)
                    for c in range(NCHK):
                        for hh in range(2):
                            K.tr(pbv[:, c * 2 + hh, :], src[:, hh, c * CH:(c + 1) * CH], identb[0:64, 0:64])
                    K.cp("act", dstT_[:], pbv)
                for (lt, dm, dn) in ((BT, MAB, NBR), (KT, MKA, NKR)):
                    for half in range(2):
                        pb = bank()
                        pbv = v3(pb[0:64, :], 128)
                        for j in range(4):
                            idx = half * 4 + j
                            c, hh = idx // 2, idx % 2
                            K.mm(pbv[:, j, :], lt[:, hh, c * CH:(c + 1) * CH], ATRT[:, hh, c, :])
                        K.tt("dve", dm[:, half * 4:half * 4 + 4, :], pbv[:, :, 0:64], v3(trisb[:, 0:256], 64), ALU.mult)
                        K.tt("dve", dn[:, half * 4:half * 4 + 4, :], pbv[:, :, 64:128], v3(triib[:, 0:256], 64), ALU.mult)
                pb = bank()
                pbv = v3(pb[0:64, :], 64)
                for idx in range(8):
                    c, hh = idx // 2, idx % 2
                    K.mm(pbv[:, idx, :], ATRT[:, hh, c, 0:64], BT[:, hh, c * CH:(c + 1) * CH])
                K.tt("dve", MABT[:], pbv, v3(tristb[:], 64), ALU.mult)
                P_, PT_, X_ = MAB, MABT, Xb[0]
                K.tt("pool", X_[:], MAB[:], v3(idrb[:], 64), ALU.add)
                for lvl in range(5):
                    last = lvl == 4
                    PTn, Pn, Xn = PTb[lvl % 2], Pb[lvl % 2], Xb[(lvl + 1) % 2]
                    pb1 = bank()
                    pv1 = v3(pb1[0:64, :], 64)
                    for idx in range(8):
                        K.mm(pv1[:, idx, :], P_[:, idx, :], PT_[:, idx, :])
                    K.cp("act", PTn[:], pv1)
                    if not last:
                        pb2 = bank()
                        pv2 = v3(pb2[0:64, :], 64)
                        for idx in range(8):
                            K.mm(pv2[:, idx, :], PT_[:, idx, :], P_[:, idx, :])
                        K.cp("dve", Pn[:], pv2)
                    pb3 = bank()
                    pv3 = v3(pb3[0:64, :], 64)
                    for idx in range(8):
                        K.mm(pv3[:, idx, :], PTn[:, idx, :], X_[:, idx, :])
                    K.tt("dve", Xn[:], pv3, X_[:], ALU.add)
                    P_, PT_, X_ = Pn, PTn, Xn
                Tm = X_
                pb = bank()
                pbv = v3(pb[0:64, :], 64)
                for idx in range(8):
                    K.mm(pbv[:, idx, :], MKA[:, idx, :], VT[:, idx, :])
                K.cp("act", W2[:], pbv)
                sW1 = v3(pseq[:, 0:128], 64)
                sUT = v3(pseq[:, 128:256], 64)
                sSN = v3(pseq[:, 256:384], 64)
                for c in range(NCHK):
                    cs = slice(c * CH, (c + 1) * CH)
                    for hh in range(2):
                        S.emit("pe", "matmul", sW1[:, hh, :], ATRT[:, hh, c, 0:64], ST16[:, hh, :], start=True, stop=True,
                               ins=[ATRT[:], ST16[:]], outs=[bW1])
                    S.emit("dve", "tensor_tensor", WS[:], sW1, W2[:, 2 * c:2 * c + 2, :], ALU.add,
                           ins=[bW1, W2[:]], outs=[WS[:]])
                    for hh in range(2):
                        S.emit("pe", "matmul", sUT[:, hh, :], Tm[:, 2 * c + hh, :], WS[:, hh, :], start=True, stop=True,
                               ins=[Tm[:], WS[:]], outs=[bUT])
                    S.emit("act", "activation", UT16[:], sUT, AF.Copy, ins=[bUT], outs=[UT16[:]])
                    for hh in range(2):
                        K.mm(pY[:, hh, cs], ST16[:, hh, :], ATRT[:, hh, c, 64:128], start=True, stop=False)
                        K.mm(pY[:, hh, cs], VT[:, 2 * c + hh, :], NKR[:, 2 * c + hh, :], start=False, stop=False)
                        K.mm(pY[:, hh, cs], UT16[:, hh, :], NBR[:, 2 * c + hh, :], start=False, stop=True)
                    for hh in range(2):
                        S.emit("pe", "matmul", sSN[:, hh, :], KHT[:, 2 * c + hh, :], VT[:, 2 * c + hh, :], start=True, stop=False,
                               ins=[KHT[:], VT[:]], outs=[bSN])
                        S.emit("pe", "matmul", sSN[:, hh, :], BHT[:, 2 * c + hh, :], UT16[:, hh, :], start=False, stop=True,
                               ins=[BHT[:], UT16[:], bSN], outs=[bSN])
                    for hh in range(2):
                        S.emit("dve", "scalar_tensor_tensor", ST32[:, hh, :], ST32[:, hh, :],
                               EP[:, hh, c * CH + CH - 1:c * CH + CH], sSN[:, hh, :], ALU.mult, ALU.add,
                               ins=[ST32[:], EP[:], bSN], outs=[ST32[:]])
                    K.cp("act", ST16[:], ST32[:])
                if not rev:
                    K.cp("act", YS[:], pY[:])
                    S.dma("sp", [(yf_d[:, :, t0:t0 + TILE], YS[:])], "yfs")
                else:
                    K.tt("dve", YS[:], pY[:], YF[:, :, ::-1], ALU.add)
                    flat = lambda ap: ap.rearrange("p h t -> p (h t)")
                    pm_ = bank()
                    K.mm(pm_[0:64, :], meanm[:], flat(YS[:]))
                    K.tt("dve", flat(YC[:]), flat(YS[:]), pm_[0:64, :], ALU.subtract)
                    K.tt("pool", SQ[:], YC[:], YC[:], ALU.mult)
                    pv_ = bank()
                    K.mm(pv_[0:64, :], meanm[:], flat(SQ[:]))
                    K.rsqrt(flat(RS[:]), pv_[0:64, :], 64e-5)
                    K.tt("pool", YC[:], YC[:], RS[:], ALU.mult)
                    for hh in range(2):
                        K.ts("dve", YL[:, hh, :], YC[:, hh, :], ptc("lnw%d" % hh), ptc("lnb%d" % hh), ALU.mult, ALU.add)
                    K.tt("pool", RK[:], Rr[:], Kr[:], ALU.mult)
                    for hh in range(2):
                        K.ts("dve", RK[:, hh, :], RK[:, hh, :], ptc("rk%d" % hh), None, ALU.mult)
                    pb_ = bank()
                    K.mm(pb_[0:64, :], onesf, flat(RK[:]))
                    K.tt("dve", flat(BON[:]), pb_[0:64, :], flat(Vr[:]), ALU.mult)
                    pg_ = bank()
                    pgv = v3(pg_[0:64, :], TILE)
                    for hh in range(2):
                        K.mm(pgv[:, hh, :], g2ab[:, hh * 64:(hh + 1) * 64], SGa[:], start=True, stop=False)
                        K.mm(pgv[:, hh, :], g2bb[:, hh * 64:(hh + 1) * 64], SGb[:], start=False, stop=True)
                    K.tt("pool", YL[:], YL[:], BON[:], ALU.add)
                    K.tt("dve", OUTB[:, :, ::-1], YL[:], pgv, ALU.mult)
                    S.dma("sp", [(ybounce_d[0:128, t0:t0 + TILE].rearrange("(h c) t -> c h t", c=64), OUTB[:])], "ybs")

            order = list(range(NT - 1, -1, -1)) if rev else list(range(NT))
            prep_tile(order[0])
            for j, i in enumerate(order):
                if j + 1 < NT:
                    prep_tile(order[j + 1])
                process_tile(i)
            S.flush()

    def attention_phase():
        alloc_att()
        run_pass(2)
        qT, kT, Vaug = AT["qT"], AT["kT"], AT["Vaug"]
        with ExitStack() as a_:
            T = lambda name, shape, dt=F32: S.sb(a_, "at_" + name, shape, dt)
            MASK = T("MASK", [128, NKB, 512], BF16)
            mst = [T("mst%d" % i, [128, 512]) for i in range(2)]
            E = [T("E%d" % i, [128, 512], BF16) for i in range(3)]
            EMk = [T("EMk%d" % i, [128, 512], BF16) for i in range(3)]
            OTs = T("OTs", [65, 512])
            RC = T("RC", [64, 512])
            YA = [T("YA%d" % i, [64, 512], BF16) for i in range(2)]
            pS = [S.ps(a_, "at_pS%d" % i, [128, 512]) for i in range(3)]
            pO = [S.ps(a_, "at_pO%d" % i, [128, 512]) for i in range(2)]
            pD = S.ps(a_, "at_pD", [64, 512])
            for off in range(NKB):
                S.dma("sp", [(mst[off % 2][:], amask_d[:, off, :])], "am%d" % (off % 2))
                K.cp("dve" if off % 2 == 0 else "pool", MASK[:, off, :], mst[off % 2][:])
            it = 0
            n_ = 0
            for hh in range(2):
                hs = slice(hh * 64, (hh + 1) * 64)
                for qt in range(SEQ // 512):
                    q0 = qt * 512
                    kbs = [kb for kb in range(q0 // 128 - 8, q0 // 128 + 12) if 0 <= kb < SEQ // 128]
                    pO_ = pO[it % 2]
                    for n, kb in enumerate(kbs):
                        off = kb - q0 // 128 + 8
                        pS_ = pS[n_ % 3]
                        E_ = E[n_ % 3]
                        EM_ = EMk[n_ % 3]
                        K.mm(pS_[:], kT[hs, kb * 128:(kb + 1) * 128], qT[hs, q0:q0 + 512])
                        K.act(E_[:], pS_[:], AF.Exp, scale=0.125)
                        K.tt("dve" if n_ % 2 == 0 else "pool", EM_[:], E_[:], MASK[:, off, :], ALU.mult)
                        K.mm(pO_[0:65, :], Vaug[:, kb, hh, :], EM_[:], start=(n == 0), stop=(n == len(kbs) - 1))
                        n_ += 1
                    K.cp("act", OTs[:], pO_[0:65, :])
                    K.mm(pD[:], sel65, OTs[:])
                    K.recip(RC[:], pD[:])
                    ya = YA[it % 2]
                    K.tt("dve", ya[:], OTs[0:64, :], RC[:], ALU.mult)
                    S.dma("sp", [(ybounce_d[128 + hh * 64:128 + hh * 64 + 64, q0:q0 + 512], ya[:])], "yas")
                    it += 1
            if L is None:
                S.collective("AllGather", ALU.bypass, ybounce_t, yg_t, "ag1")
            S.flush()
        att.close()

    AXX = mybir.AxisListType.X

    def phase4():
        OH = sb("OH", [128, 8])
        gffn = sb("gffn_sb", [128, DC])
        if L is None:
            S.dma("sp", [(OH[:], oh_d), (gffn[:], gffn_d)], "p4c")
        else:
            S.dma("sp", [(gffn[:], gffn_d)], "p4c")
        with ExitStack() as p4:
            YTs = S.sb(p4, "YTs", [128, DC, TOK_PER_CORE], BF16)
            with ExitStack() as p4a:
                if L is not None:
                    S.dma("sp", [(YTs[:], yt_in.rearrange("(kc p) t -> p kc t", p=128))], "ytl")
                blk = [S.sb(p4a, "yblk%d" % i, [128, DC, TOK_PER_CORE], BF16) for i in range(2)] if L is None else []
                for j in range(NCORE if L is None else 0):
                    b = blk[j % 2]
                    S.dma("sp", [(b[:], yg_d[:, j * TOK_PER_CORE:(j + 1) * TOK_PER_CORE].rearrange("(kc p) t -> p kc t", p=128))],
                          "yb%d" % (j % 2))
                    for half in range(2):
                        hs = slice(half * 8, half * 8 + 8)
                        if j == 0:
                            K.ts("dve", YTs[:, hs, :], b[:, hs, :], OH[:, 0:1], None, ALU.mult)
                        else:
                            K.stt(YTs[:, hs, :], b[:, hs, :], OH[:, j:j + 1], YTs[:, hs, :], ALU.mult, ALU.add)
                S.flush()
            import os
            P4STOP = int(os.environ.get("P4STOP", "9"))
            if P4STOP <= 1:
                return OH, gffn
            T = lambda name, shape, dt=F32: S.sb(p4, "p4_" + name, shape, dt)
            WOb = T("WOb", [128, DC, D], BF16)
            WRf = T("WRf", [128, DC, 36])
            BR = T("BR", [128, 36])
            xtl = [T("xt%d" % i, [128, D]) for i in range(2)]
            wos = xtl
            X2 = [T("X2_%d" % i, [128, D]) for i in range(2)]
            junk = T("junk", [128, D], BF16)
            H2f = T("H2f", [128, D])
            Hhi = T("Hhi", [128, D], BF16)
            Hlo = T("Hlo", [128, D], BF16)
            h2Tl = T("h2Tl", [128, DC, 128], BF16)
            WRhi = T("WRhi", [128, DC, 36], BF16)
            WRlo = T("WRlo", [128, DC, 36], BF16)
            h2Tb = T("h2Tb", [128, DC, TOK_PER_CORE], BF16)
            sm = lambda name, n: T(name, [128, n])
            ssq, rstd = sm("ssq", 1), sm("rstd", 1)
            LG, gmax, ngmax, GOH, EG, gsum, g1 = sm("L", 36), sm("gmax", 1), sm("ngmax", 1), sm("GOH", 4), sm("EG", 4), sm("gsum", 1), sm("g1", 1)
            el, m1, OH1, el2, m2, OH2 = sm("el", 8), sm("m1", 1), sm("OH1", 8), sm("el2", 8), sm("m2", 1), sm("OH2", 8)
            dm_, e2, den, w1, w2_, W12, WGt = sm("dm", 1), sm("e2", 1), sm("den", 1), sm("w1", 1), sm("w2", 1), sm("W12", 8), sm("WGt", 8)
            COMB = [T("COMB%d" % i, [128, 32]) for i in range(2)]
            pp = [S.ps(p4, "p4_pp%d" % i, [128, 512]) for i in range(4)]
            ptp = [S.ps(p4, "p4_pt%d" % i, [128, 8, 128], BF16) for i in range(2)]
            pl = S.ps(p4, "p4_pl", [128, 64])
            identf = cst[:, C_IDENT:C_IDENT + 128]
            for kc in range(DC):
                r0 = (kc % 2) * 1024 + (kc // 2) * 128
                ws = wos[kc % 2]
                S.dma("sp", [(ws[:], wout_d[r0:r0 + 128, :])], "wo%d" % (kc % 2))
                K.cp("dve" if kc % 2 == 0 else "pool", WOb[:, kc, :], ws[:])
            S.dma("sp", [(WRf[:], wr_d.rearrange("(dc p) n -> p dc n", p=128)), (BR[:], br_d.partition_broadcast(128))], "wr")
            for dc in range(DC):
                K.ts("dve", WRf[:, dc, :], WRf[:, dc, :], gffn[:, dc:dc + 1], None, ALU.mult)
            K.cp("dve", WRhi[:], WRf[:])
            K.tt("dve", WRlo[:], WRf[:], WRhi[:], ALU.subtract)
            nb = 0
            if P4STOP <= 2:
                S.flush()
                return OH, gffn
            for tb in range(TOK_PER_CORE // 128):
                xt_ = xtl[tb % 2]
                x2 = X2[tb % 2]
                S.dma("sp", [(xt_[:], xs_d[tb * 128:(tb + 1) * 128, :])], "xs%d" % (tb % 2))
                for n4 in range(4):
                    pb = pp[nb % 4]
                    nb += 1
                    for kc in range(DC):
                        K.mm(pb[:], YTs[:, kc, tb * 128:(tb + 1) * 128], WOb[:, kc, n4 * 512:(n4 + 1) * 512],
                             start=(kc == 0), stop=(kc == DC - 1))
                    K.tt("dve", x2[:, n4 * 512:(n4 + 1) * 512], pb[:], xt_[:, n4 * 512:(n4 + 1) * 512], ALU.add)
                S.dma("sp", [(x2s_d[tb * 128:(tb + 1) * 128, :], x2[:])], "x2s")
                if debug:
                    S.dma("sp", [(dbg["x2"][tb * 128:(tb + 1) * 128, :], x2[:])], "x2d")
                if P4STOP <= 3:
                    continue
                K.act(junk[:], x2[:], AF.Square, accum=ssq[:])
                K.ts("dve", rstd[:], ssq[:], 1.0 / D, 1e-6, ALU.mult, ALU.add)
                K.act(rstd[:], rstd[:], AF.Sqrt)
                K.recip(rstd[:], rstd[:])
                K.ts("pool", H2f[:], x2[:], rstd[:], None, ALU.mult)
                K.cp("act", Hhi[:], H2f[:])
                K.tt("dve", Hlo[:], H2f[:], Hhi[:], ALU.subtract)
                tcols = slice(tb * 128, (tb + 1) * 128)
                nq = 0
                for (src, dstf) in ((Hhi, lambda h8: h2Tb[:, h8 * 8:h8 * 8 + 8, tcols]), (Hlo, lambda h8: h2Tl[:, h8 * 8:h8 * 8 + 8, :])):
                    for h8 in range(2):
                        pt_ = ptp[nq % 2]
                        nq += 1
                        for j in range(8):
                            dc = h8 * 8 + j
                            K.tr(pt_[:, j, :], src[:, dc * 128:(dc + 1) * 128], identb[:])
                        K.cp("act" if h8 == 0 else "dve", dstf(h8), pt_[:])
                if P4STOP <= 4:
                    continue
                nmm = 0
                for dc in range(DC):
                    for (lt, rt) in ((h2Tb[:, dc, tcols], WRhi), (h2Tb[:, dc, tcols], WRlo), (h2Tl[:, dc, :], WRhi)):
                        K.mm(pl[:, 0:36], lt, rt[:, dc, :], start=(nmm == 0), stop=(nmm == 3 * DC - 1))
                        nmm += 1
                K.tt("dve", LG[:], pl[:, 0:36], BR[:], ALU.add)
                red = lambda out, in_: S.emit("dve", "reduce_max", out, in_, AXX, ins=[in_], outs=[out])
                red(gmax[:], LG[:, 0:4])
                K.ts("dve", GOH[:], LG[:, 0:4], gmax[:], None, ALU.is_equal)
                K.ts("dve", ngmax[:], gmax[:], -1.0, None, ALU.mult)
                K.act(EG[:], LG[:, 0:4], AF.Exp, bias=ngmax[:], accum=gsum[:])
                K.recip(g1[:], gsum[:])
                K.ts("dve", el[:], LG[:, 4:12], GOH[:, 0:1], None, ALU.mult)
                for g in range(1, 4):
                    K.stt(el[:], LG[:, 4 + 8 * g:12 + 8 * g], GOH[:, g:g + 1], el[:], ALU.mult, ALU.add)
                red(m1[:], el[:])
                K.ts("dve", OH1[:], el[:], m1[:], None, ALU.is_equal)
                K.stt(el2[:], OH1[:], -1e30, el[:], ALU.mult, ALU.add)
                red(m2[:], el2[:])
                K.ts("dve", OH2[:], el2[:], m2[:], None, ALU.is_equal)
                K.tt("dve", dm_[:], m2[:], m1[:], ALU.subtract)
                K.act(e2[:], dm_[:], AF.Exp)
                K.ts("dve", den[:], e2[:], 1.0, None, ALU.add)
                K.recip(w1[:], den[:])
                K.tt("dve", w2_[:], e2[:], w1[:], ALU.mult)
                K.ts("dve", W12[:], OH1[:], w1[:], None, ALU.mult)
                K.stt(W12[:], OH2[:], w2_[:], W12[:], ALU.mult, ALU.add)
                K.ts("dve", WGt[:], W12[:], g1[:], None, ALU.mult)
                cm = COMB[tb % 2]
                for g in range(4):
                    K.ts("dve", cm[:, g * 8:(g + 1) * 8], WGt[:], GOH[:, g:g + 1], None, ALU.mult)
                S.dma("sp", [(cb_d[tb * 128:(tb + 1) * 128, :], cm[:])], "cbs")
                if debug:
                    S.dma("sp", [(dbg["comb"][tb * 128:(tb + 1) * 128, :], cm[:])], "cbd")
            S.dma("sp", [(hb_d.rearrange("(dc p) t -> p dc t", p=128), h2Tb[:])], "hbs")
            import os
            if L is None and not os.environ.get("DBG_NOCC2"):
                S.collective("AllGather", ALU.bypass, hb_t, hg_t, "ag2")
                S.collective("AllGather", ALU.bypass, cb_t, cg_t, "ag3")
            S.flush()
        return OH, gffn

    def phase5(OH, gffn):
        with ExitStack() as p5:
            T = lambda name, shape, dt=F32: S.sb(p5, "p5_" + name, shape, dt)
            with ExitStack() as p5a:
                st_ = [S.sb(p5a, "p5_wst%d" % i, [128, D], F32) for i in range(3)]
                ob_ = [S.sb(p5a, "p5_wob%d" % i, [128, D], BF16) for i in range(3)]
                n = 0
                for (src, dst) in ((wg_d, wg16_d), (wu_d, wu16_d)):
                    for e in range(4):
                        for dc in range(0, DC, 2):
                            s_, o_ = st_[n % 3], ob_[n % 3]
                            S.dma("sp", [(s_[:, 0:1024], src[e, dc * 128:(dc + 1) * 128, :]),
                                         (s_[:, 1024:2048], src[e, (dc + 1) * 128:(dc + 2) * 128, :])], "ws%d" % (n % 3))
                            for k2 in range(2):
                                eng = ("dve", "pool")[(n + k2) % 2]
                                K.ts(eng, o_[:, k2 * 1024:(k2 + 1) * 1024], s_[:, k2 * 1024:(k2 + 1) * 1024],
                                     gffn[:, dc + k2:dc + k2 + 1], None, ALU.mult)
                            S.dma("sp", [(dst[e, :, dc:dc + 2, :], o_[:].rearrange("p (a b) -> p a b", b=1024))], "wt%d" % (n % 3))
                            n += 1
                for e in range(4):
                    for hc in range(8):
                        s_, o_ = st_[n % 3], ob_[n % 3]
                        S.dma("sp", [(s_[:], wd_d[e, hc * 128:(hc + 1) * 128, :])], "ws%d" % (n % 3))
                        if n % 3 == 2:
                            K.cp("act", o_[:], s_[:])
                        else:
                            K.cp(("dve", "pool")[n % 2], o_[:], s_[:])
                        S.dma("sp", [(wd16_d[e, :, hc, :], o_[:])], "wt%d" % (n % 3))
                        n += 1
                S.flush()
            CW = T("CW", [128, SEQ // 128, 4])
            if L is None:
                CG = T("CG", [128, SEQ // 128, 32])
                S.dma("sp", [(CG[:], cg_d.rearrange("(tb p) e -> p tb e", p=128))], "cgl")
            else:
                S.dma("sp", [(CW[:], cw_in.rearrange("(tb p) e -> p tb e", p=128))], "cgl")
            for c in range(NCORE if L is None else 0):
                if c == 0:
                    K.ts("dve", CW[:], CG[:, :, 0:4], OH[:, 0:1], None, ALU.mult)
                else:
                    K.stt(CW[:], CG[:, :, 4 * c:4 * c + 4], OH[:, c:c + 1], CW[:], ALU.mult, ALU.add)
            H = [T("H%d" % i, [128, DC, 512], BF16) for i in range(2)]
            GW_ = [T("GW%d" % i, [128, DC, 256], BF16) for i in range(2)]
            UW_ = [T("UW%d" % i, [128, DC, 256], BF16) for i in range(2)]
            DW_ = [T("DW%d" % i, [128, 8, 512], BF16) for i in range(3)]
            hidT = [T("hidT%d" % i, [128, 8, 512], BF16) for i in range(2)]
            SG = [T("SG%d" % i, [128, 512]) for i in range(2)]
            YP = [T("YP%d" % i, [128, 4, D]) for i in range(2)]
            pgu = [S.ps(p5, "p5_pgu%d" % i, [128, 512]) for i in range(4)]
            pdn = [S.ps(p5, "p5_pdn%d" % i, [128, 512]) for i in range(4)]
            ngu = ndn = nw = ndw = nh = 0
            for j in range(SEQ // 512):
                jc, half = j // 2, j % 2
                Hj = H[j % 2]
                S.dma("sp", [(Hj[:], hg_d[jc * D:(jc + 1) * D, half * 512:(half + 1) * 512].rearrange("(dc p) t -> p dc t", p=128))],
                      "hl%d" % (j % 2))
                yp = YP[j % 2]
                for e in range(4):
                    hid = hidT[nh % 2]
                    nh += 1
                    for piece in range(4):
                        gw, uw = GW_[nw % 2], UW_[nw % 2]
                        S.dma("sp", [(gw[:], wg16_d[e, :, :, piece * 256:(piece + 1) * 256])], "gw%d" % (nw % 2))
                        S.dma("sp", [(uw[:], wu16_d[e, :, :, piece * 256:(piece + 1) * 256])], "uw%d" % (nw % 2))
                        nw += 1
                        for hcl in range(2):
                            hc = piece * 2 + hcl
                            pg_, pu_ = pgu[ngu % 4], pgu[(ngu + 1) % 4]
                            ngu += 2
                            for dc in range(DC):
                                K.mm(pg_[:], gw[:, dc, hcl * 128:(hcl + 1) * 128], Hj[:, dc, :], start=(dc == 0), stop=(dc == DC - 1))
                            for dc in range(DC):
                                K.mm(pu_[:], uw[:, dc, hcl * 128:(hcl + 1) * 128], Hj[:, dc, :], start=(dc == 0), stop=(dc == DC - 1))
                            sg = SG[hc % 2]
                            K.act(sg[:], pg_[:], AF.Silu)
                            K.tt("dve", hid[:, hc, :], sg[:], pu_[:], ALU.mult)
                    for n4 in range(4):
                        dw = DW_[ndw % 3]
                        ndw += 1
                        S.dma("sp", [(dw[:], wd16_d[e, :, :, n4 * 512:(n4 + 1) * 512])], "dw%d" % (ndw % 3))
                        for tb in range(4):
                            pd_ = pdn[ndn % 4]
                            ndn += 1
                            for hc in range(8):
                                K.mm(pd_[:], hid[:, hc, tb * 128:(tb + 1) * 128], dw[:, hc, :], start=(hc == 0), stop=(hc == 7))
                            cw = CW[:, j * 4 + tb, e:e + 1]
                            dst = yp[:, tb, n4 * 512:(n4 + 1) * 512]
                            if e == 0:
                                K.ts("dve", dst, pd_[:], cw, None, ALU.mult)
                            else:
                                K.stt(dst, pd_[:], cw, dst, ALU.mult, ALU.add)
                S.dma("sp", [(yp_d[j * 512:(j + 1) * 512, :].rearrange("(tb p) n -> p tb n", p=128), yp[:])], "yps")
            if L is None:
                S.collective("ReduceScatter", ALU.add, yp_t, ym_t, "rs1")
            S.flush()

    def phase6():
        with ExitStack() as p6:
            T = lambda name, shape, dt=F32: S.sb(p6, "p6_" + name, shape, dt)
            GF = T("GF", [128, D])
            S.dma("sp", [(GF[:], gfin_d.partition_broadcast(128))], "gf")
            xa = [T("xa%d" % i, [128, D]) for i in range(2)]
            ya_ = [T("ya%d" % i, [128, D]) for i in range(2)]
            junk = T("junk", [128, D], BF16)
            ssq = [T("ssq%d" % i, [128, 1]) for i in range(2)]
            fin = []
            for tb in range(TOK_PER_CORE // 128):
                a, b = xa[tb % 2], ya_[tb % 2]
                if L is None:
                    S.dma("sp", [(a[:], x2s_d[tb * 128:(tb + 1) * 128, :]), (b[:], ym_d[tb * 128:(tb + 1) * 128, :])], "f%d" % (tb % 2))
                    K.tt("dve", a[:], a[:], b[:], ALU.add)
                else:
                    S.dma("sp", [(a[:], x2s_d[tb * 128:(tb + 1) * 128, :])], "f%d" % (tb % 2))
                    for c in range(NCORE):
                        S.dma("sp", [(b[:], ypi_d[c, tb * 128:(tb + 1) * 128, :])], "g%d" % (tb % 2))
                        K.tt("dve" if c % 2 == 0 else "pool", a[:], a[:], b[:], ALU.add)
                q = ssq[tb % 2]
                K.act(junk[:], a[:], AF.Square, accum=q[:])
                K.ts("dve", q[:], q[:], 1.0 / D, 1e-6, ALU.mult, ALU.add)
                K.act(q[:], q[:], AF.Sqrt)
                K.recip(q[:], q[:])
                K.ts("pool", b[:], a[:], q[:], None, ALU.mult)
                K.tt("dve", b[:], b[:], GF[:], ALU.mult)
                S.dma("sp", [(out_d[tb * 128:(tb + 1) * 128, :], b[:])], "out")
            S.flush()

    import os
    SKIP123 = bool(os.environ.get("DBG_SKIP123"))
    if upto >= 1 and not SKIP123 and L in (None, 1):
        run_pass(0)
    if upto >= 2 and not SKIP123 and L in (None, 1):
        run_pass(1)
    if upto >= 3 and not SKIP123 and L in (None, 1):
        attention_phase()
    if upto >= 4 and L in (None, 2):
        OH_, gffn_ = phase4()
    if L == 3:
        OH_ = None
        gffn_ = sb("gffn_sb", [128, DC])
        S.dma("sp", [(gffn_[:], gffn_d)], "p4c")
    if upto >= 5 and L in (None, 3):
        phase5(OH_, gffn_)
    if upto >= 6 and L in (None, 4):
        phase6()
    if debug and upto < 4:
        with ExitStack() as dstk:
            dtile = S.sb(dstk, "dbgt", [128, 2048], F32)
            fin = []
            if upto >= 3:
                fin.append(S.dma("sp", [(dbg["yb"][:, :], yg_d[256:512, :])], "dbg0"))
            elif upto >= 2:
                fin.append(S.dma("sp", [(dbg["yb"][0:128, :], ybounce_d[0:128, :])], "dbg0"))
            fin.append(S.dma("sp", [(dbg["yf"], yf_d)], "dbg1"))
            S.flush()
    return nc, K, dict(st=st, att=att, dbg=dbg, yf_d=yf_d, ybounce_d=ybounce_d, yg_t=yg_t, ybounce_t=ybounce_t,
                       AT=AT, alloc_att=alloc_att, run_pass=run_pass, out_d=out_d, cst=cst, sel65=sel65, sb=sb,
                       amask_d=amask_d, x_d=x_d, declared=declared, wout_d=wout_d, wr_d=wr_d, br_d=br_d, gffn_d=gffn_d, gfin_d=gfin_d,
                       wg_d=wg_d, wu_d=wu_d, wd_d=wd_d, identb=identb)


_CACHE = {}
FUSED = False


def _run(launch, ims_all):
    if launch not in _CACHE:
        nc, K, ctx = build(upto=6, debug=False, launch=launch)
        _CACHE[launch] = (nc, ctx["declared"])
    nc, declared = _CACHE[launch]
    ims = [{k: m[k] for k in declared} for m in ims_all]
    return run_bass_kernel_spmd(nc, ims, core_ids=list(range(NCORE))).results


def kernel(**inputs):
    maps = prep_inputs(inputs)
    if FUSED:
        res = _run(None, maps)
        out = np.concatenate([np.asarray(res[c]["out"]) for c in range(NCORE)], axis=0)
        return out.reshape(1, SEQ, D).astype(np.float32)
    r1 = _run(1, maps)
    yg = np.concatenate([np.asarray(r1[c]["ybo"]) for c in range(NCORE)], axis=0)
    for c in range(NCORE):
        maps[c]["yt"] = np.ascontiguousarray(yg[:, c * TOK_PER_CORE:(c + 1) * TOK_PER_CORE])
    r2 = _run(2, maps)
    hg = np.concatenate([np.asarray(r2[c]["hbo"]) for c in range(NCORE)], axis=0)
    cw_all = np.concatenate([np.asarray(r2[c]["cbo"]) for c in range(NCORE)], axis=0)
    for c in range(NCORE):
        maps[c]["hg"] = hg
        maps[c]["cwi"] = np.ascontiguousarray(cw_all[:, 4 * c:4 * c + 4])
    r3 = _run(3, maps)
    for c in range(NCORE):
        maps[c]["ypi"] = np.stack([np.asarray(r3[k]["ypo"])[c * TOK_PER_CORE:(c + 1) * TOK_PER_CORE] for k in range(NCORE)])
        maps[c]["x2i"] = np.asarray(r2[c]["x2o"])
    r4 = _run(4, maps)
    out = np.concatenate([np.asarray(r4[c]["out"]) for c in range(NCORE)], axis=0)
    return out.reshape(1, SEQ, D).astype(np.float32)
```

```python
import numpy as np
import concourse.bass as bass
import concourse.mybir as mybir
from concourse.bass_utils import run_bass_kernel_spmd
from contextlib import ExitStack

F32 = mybir.dt.float32
BF16 = mybir.dt.bfloat16
I32 = mybir.dt.int32
AF = mybir.ActivationFunctionType
ALU = mybir.AluOpType

NCORE = 8
SEQ = 8192
D = 2048
DC = 16
TILE = 256
NT = SEQ // TILE
CH = 64
NCHK = TILE // CH
TOK_PER_CORE = SEQ // NCORE
ENGS = ("pe", "act", "dve", "pool", "sp")


class Buf:
    __slots__ = ("name", "w", "r")

    def __init__(self, name):
        self.name = name
        self.w = None
        self.r = {}


class Op:
    __slots__ = ("eng", "fns", "deps", "signal", "val", "is_dma", "semkey", "inc")

    def __init__(self, eng, fns, is_dma=False, semkey=None, inc=16):
        self.eng = eng
        self.fns = fns
        self.deps = {}
        self.signal = False
        self.val = None
        self.is_dma = is_dma
        self.semkey = semkey
        self.inc = inc


def _isap(x):
    return hasattr(x, "tensor")


class Sched:
    def __init__(self, nc):
        self.nc = nc
        self.es = ExitStack()
        self.sems = {}
        self.cnt = {}
        self.waited = {e: {} for e in ENGS}
        self.engobj = {"pe": nc.tensor, "act": nc.scalar, "dve": nc.vector,
                       "pool": nc.gpsimd, "sp": nc.sync}
        self.nops = {e: 0 for e in ENGS}
        self.reset()

    def reset(self):
        self.bufs = {}
        self.streams = {e: [] for e in ENGS}
        self.keymap = {}
        self.seg = None

    def _commit(self, op, ins, outs):
        if self.seg is not None:
            self.seg.append((op, ins, outs))
            return op
        self._track(op, ins, outs)
        self.streams[op.eng].append(op)
        return op

    def commit_merged(self, *segs):
        assert self.seg is None
        segs = [sg for sg in segs if sg]
        pos = [0] * len(segs)
        total = sum(len(sg) for sg in segs)
        for _ in range(total):
            best, bf = None, None
            for q, sg in enumerate(segs):
                if pos[q] < len(sg):
                    f = pos[q] / len(sg)
                    if bf is None or f < bf:
                        best, bf = q, f
            self._commit(*segs[best][pos[best]])
            pos[best] += 1

    def sb(self, stack, name, shape, dtype):
        return stack.enter_context(self.nc.sbuf_tensor(name, list(shape), dtype))

    def ps(self, stack, name, shape, dtype=F32):
        return stack.enter_context(self.nc.psum_tensor(name, list(shape), dtype))

    def buf_of(self, a):
        if isinstance(a, Buf):
            return a
        if isinstance(a, str):
            name = a
        else:
            name = a.tensor.name
        b = self.bufs.get(name)
        if b is None:
            b = self.bufs[name] = Buf(name)
        return b

    def _track(self, op, ins, outs):
        for a in ins:
            b = self.buf_of(a)
            if b.w is not None and b.w is not op:
                op.deps[b.w] = True
        for a in outs:
            b = self.buf_of(a)
            if b.w is not None and b.w is not op:
                op.deps.setdefault(b.w, False)
            for r in b.r.values():
                if r is not op:
                    op.deps.setdefault(r, False)
        for a in ins:
            self.buf_of(a).r[id(op) if op.is_dma else op.eng] = op
        for a in outs:
            b = self.buf_of(a)
            b.w = op
            b.r = {}

    def emit(self, eng, meth, *args, ins=(), outs=(), **kw):
        def fn(e, meth=meth, args=args, kw=kw):
            return getattr(e, meth)(*args, **kw)
        op = Op(eng, [fn])
        return self._commit(op, list(ins), list(outs))

    def dma(self, q, pairs, semkey, ins=(), outs=(), **kw):
        fns = []
        for (o, i) in pairs:
            def fn(e, o=o, i=i, kw=kw):
                return e.dma_start(out=o, in_=i, **kw)
            fns.append(fn)
        ins = list(ins) + [i for (o, i) in pairs]
        outs = list(outs) + [o for (o, i) in pairs]
        op = Op(q, fns, is_dma=True, semkey=semkey)
        return self._commit(op, ins, outs)

    def collective(self, kind, alu, in_t, out_t, semkey):
        def fn(e):
            return e.collective_compute(kind, alu, replica_groups=[list(range(NCORE))],
                                        ins=[in_t.ap().opt()], outs=[out_t.ap().opt()])
        op = Op("pool", [fn], is_dma=True, semkey=semkey, inc=1)
        self._track(op, [in_t.ap()], [out_t.ap()])
        self.streams["pool"].append(op)
        return op

    def _key(self, op):
        if not op.is_dma:
            return "e_" + op.eng
        if op.inc == 1:
            return "c_cc"
        k = self.keymap.get(op.semkey)
        if k is None:
            k = self.keymap[op.semkey] = "d_%d" % len(self.keymap)
        return k

    def _sem(self, key):
        s = self.sems.get(key)
        if s is None:
            s = self.sems[key] = self.es.enter_context(self.nc.semaphore(key))
        return s

    def flush(self, final=False):
        nc = self.nc
        for eng, s in self.streams.items():
            for op in s:
                for d, raw in op.deps.items():
                    if d.is_dma:
                        continue
                    if (not op.is_dma) and d.eng == op.eng and (op.eng == "pe" or not raw):
                        continue
                    d.signal = True
            last = None
            for op in s:
                if not op.is_dma:
                    last = op
            if last is not None:
                last.signal = True
        for eng, s in self.streams.items():
            for op in s:
                if op.is_dma:
                    k = self._key(op)
                    self.cnt[k] = self.cnt.get(k, 0) + op.inc * len(op.fns)
                    op.val = self.cnt[k]
                    self._sem(k)
                elif op.signal:
                    k = self._key(op)
                    self.cnt[k] = self.cnt.get(k, 0) + 1
                    op.val = self.cnt[k]
                    self._sem(k)
        totals = dict(self.cnt)
        with nc.Block() as block:
            def run_stream(engname, e):
                waited = self.waited[engname]
                for op in self.streams[engname]:
                    for d, raw in op.deps.items():
                        if (not d.is_dma) and (not op.is_dma) and d.eng == engname \
                                and (engname == "pe" or not raw):
                            continue
                        k = self._key(d)
                        if waited.get(k, 0) >= d.val:
                            continue
                        e.wait_ge(self.sems[k], d.val)
                        waited[k] = d.val
                    for fn in op.fns:
                        inst = fn(e)
                        if op.is_dma:
                            inst.then_inc(self.sems[self._key(op)], op.inc)
                        elif op.signal:
                            inst.then_inc(self.sems[self._key(op)], 1)
                    self.nops[engname] += len(op.fns)
                for k, v in totals.items():
                    if waited.get(k, 0) >= v:
                        continue
                    e.wait_ge(self.sems[k], v)
                    waited[k] = v

            @block.tensor
            def _(e):
                run_stream("pe", e)

            @block.scalar
            def _(e):
                run_stream("act", e)

            @block.vector
            def _(e):
                run_stream("dve", e)

            @block.gpsimd
            def _(e):
                run_stream("pool", e)

            @block.sync
            def _(e):
                run_stream("sp", e)
        self.reset()


SHIFT_GROUPS = ["r0", "r1", "k0", "k1", "v0", "v1", "wlf", "alf", "wlb", "alb", "gla", "glb"]
P1_GROUPS = ["r0", "r1", "k0", "k1", "v0", "v1", "wlf", "alf"]
P3_GROUPS = ["aq", "ak", "av"]
P2_GROUPS = ["r0", "r1", "k0", "k1", "v0", "v1", "wlb", "alb", "gla", "glb"]
GW = {"r0": 64, "r1": 64, "k0": 64, "k1": 64, "v0": 64, "v1": 64, "wlf": 64, "alf": 64,
      "wlb": 64, "alb": 64, "gla": 128, "glb": 32, "aq": 128, "ak": 128, "av": 128}


def group_offsets(groups):
    off, o = {}, 0
    for g in groups:
        off[g] = o
        o += GW[g]
    return off, o


P1_OFF, P1_N = group_offsets(P1_GROUPS)
P2_OFF, P2_N = group_offsets(P2_GROUPS)
P3_OFF, P3_N = group_offsets(P3_GROUPS)


def pt_names():
    n = []
    for g in SHIFT_GROUPS:
        n += ["mp_" + g, "mn_" + g]
    n += ["kk0", "kk1", "ka0", "ka1", "rk0", "rk1", "lnw0", "lnw1", "lnb0", "lnb1",
          "w0f0", "w0f1", "w0b0", "w0b1", "a0f0", "a0f1", "a0b0", "a0b1", "invf"]
    return n


PT_NAMES = pt_names()
PT_IDX = {n: i for i, n in enumerate(PT_NAMES)}
NPT = len(PT_NAMES)

C_IDENT = 0
C_ROTM = 128
C_TRIS = 256
C_TRII = 256 + 512
C_TRIST = 256 + 1024
C_IDR = 256 + 1536
C_ONES = 256 + 2048
C_SCAN = C_ONES + 64
C_SEL = C_SCAN + 256
C_BLK = C_SEL + 64
NCST = C_BLK + 128

NKB = 20


def make_consts():
    c = np.zeros((128, NCST), np.float32)
    c[:, C_IDENT:C_IDENT + 128] = np.eye(128, dtype=np.float32)
    rot = np.zeros((128, 128), np.float32)
    for h in range(2):
        for i in range(8):
            rot[h * 64 + 8 + i, h * 64 + i] = -1.0
            rot[h * 64 + i, h * 64 + 8 + i] = 1.0
    c[:, C_ROTM:C_ROTM + 128] = rot
    s = np.arange(64)[:, None]
    t = np.arange(64)[None, :]
    tri_s = (s < t).astype(np.float32)
    tri_i = (s <= t).astype(np.float32)
    c[:64, C_TRIS:C_TRIS + 512] = np.tile(tri_s, (1, 8))
    c[:64, C_TRII:C_TRII + 512] = np.tile(tri_i, (1, 8))
    c[:64, C_TRIST:C_TRIST + 512] = np.tile(tri_s.T, (1, 8))
    c[:64, C_IDR:C_IDR + 512] = np.tile(np.eye(64, dtype=np.float32), (1, 8))
    c[:, C_ONES:C_ONES + 64] = 1.0
    sm = np.ones((256,), np.float32)
    sm[::64] = 0.0
    c[:, C_SCAN:C_SCAN + 256] = sm[None, :]
    c[64, C_SEL:C_SEL + 64] = 1.0
    return c


def make_attn_mask():
    k = np.arange(128)[:, None, None]
    off = np.arange(NKB)[None, :, None]
    q = np.arange(512)[None, None, :]
    d = (off - 8) * 128 + k - q
    ad = np.abs(d)
    m = (ad <= 64).astype(np.float32) + ((d % 4 == 0) & (ad <= 256)).astype(np.float32) \
        + ((d % 16 == 0) & (ad <= 1024)).astype(np.float32)
    return np.ascontiguousarray(m.astype(np.float32))


def prep_inputs(inputs):
    x = np.ascontiguousarray(np.asarray(inputs["x"])[0])
    pos = np.ascontiguousarray(np.asarray(inputs["positions"])[0].astype(np.int32))
    w_in = np.asarray(inputs["w_in"])[0]
    mu = np.asarray(inputs["mu_shift"])[0]
    w0 = np.asarray(inputs["w0"])[0]
    w2 = np.asarray(inputs["w2"])[0]
    a0 = np.asarray(inputs["a0"])[0]
    a2 = np.asarray(inputs["a2"])[0]
    g2 = np.asarray(inputs["g2"])[0]
    k_k = np.asarray(inputs["k_k"])[0]
    k_a = np.asarray(inputs["k_a"])[0]
    r_k = np.asarray(inputs["r_k"])[0]
    ln_w = np.asarray(inputs["ln_x_w"])[0]
    ln_b = np.asarray(inputs["ln_x_b"])[0]
    w_out = np.ascontiguousarray(np.asarray(inputs["w_out"])[0])
    gmix = np.ascontiguousarray(np.asarray(inputs["norm_mix"])[0].reshape(DC, 128).T)
    gffn = np.ascontiguousarray(np.asarray(inputs["norm_ffn"])[0].reshape(DC, 128).T)
    gfin = np.ascontiguousarray(np.asarray(inputs["norm_final"]).reshape(D))
    rw = np.asarray(inputs["router_group_w"])[0]
    rew = np.asarray(inputs["router_expert_w"])[0]
    wr = np.ascontiguousarray(np.concatenate([rw] + [rew[g] for g in range(4)], axis=1))
    br = np.ascontiguousarray(np.concatenate([np.asarray(inputs["router_group_b"])[0].reshape(-1),
                                              np.asarray(inputs["router_expert_b"])[0].reshape(-1)]))
    wg = np.asarray(inputs["w_gate"])[0]
    wu = np.asarray(inputs["w_up"])[0]
    wd = np.asarray(inputs["w_down"])[0]
    consts = make_consts()
    amask = make_attn_mask()
    inv_freq = (500000.0 ** (-np.arange(8, dtype=np.float32) * 2.0 / 16.0)).astype(np.float32)
    maps = []
    for c in range(NCORE):
        h = (2 * c, 2 * c + 1)

        def hcol(base, hh):
            return np.arange(base + hh * 64, base + hh * 64 + 64)
        cols = {
            "r0": hcol(0, h[0]), "r1": hcol(0, h[1]), "k0": hcol(1024, h[0]), "k1": hcol(1024, h[1]),
            "v0": hcol(2048, h[0]), "v1": hcol(2048, h[1]),
            "wlf": 3072 + np.arange(64), "wlb": 3136 + np.arange(64),
            "alf": 3200 + np.arange(64), "alb": 3264 + np.arange(64),
            "gla": 3328 + np.arange(128), "glb": 3456 + np.arange(32),
            "aq": np.concatenate([hcol(3488, h[0]), hcol(3488, h[1])]),
            "ak": np.concatenate([hcol(3488 + 1024, h[0]), hcol(3488 + 1024, h[1])]),
            "av": np.concatenate([hcol(3488 + 2048, h[0]), hcol(3488 + 2048, h[1])]),
        }
        w1 = np.ascontiguousarray(w_in[:, np.concatenate([cols[g] for g in P1_GROUPS])])
        w2p = np.ascontiguousarray(w_in[:, np.concatenate([cols[g] for g in P2_GROUPS])])
        w3 = np.ascontiguousarray(w_in[:, np.concatenate([cols[g] for g in P3_GROUPS])])
        pt = np.zeros((128, NPT), np.float32)

        def put(name, vec):
            pt[:len(vec), PT_IDX[name]] = vec
        for g in SHIFT_GROUPS:
            put("mp_" + g, mu[0][cols[g]])
            put("mn_" + g, mu[1][cols[g]])
        for i in range(2):
            ch = np.arange(h[i] * 64, h[i] * 64 + 64)
            put("kk%d" % i, k_k[ch])
            put("ka%d" % i, k_a[ch])
            put("rk%d" % i, r_k[h[i]])
            put("lnw%d" % i, ln_w[ch])
            put("lnb%d" % i, ln_b[ch])
            put("w0f%d" % i, w0[0][ch])
            put("w0b%d" % i, w0[1][ch])
            put("a0f%d" % i, a0[0][ch])
            put("a0b%d" % i, a0[1][ch])
        invf = np.zeros((128,), np.float32)
        for hh in range(2):
            invf[hh * 64:hh * 64 + 8] = inv_freq
            invf[hh * 64 + 8:hh * 64 + 16] = inv_freq
        put("invf", invf)
        chs = np.concatenate([np.arange(h[0] * 64, h[0] * 64 + 64), np.arange(h[1] * 64, h[1] * 64 + 64)])
        lw = np.zeros((64, 8, 64), np.float32)
        for d_ in range(2):
            for i in range(2):
                ch = np.arange(h[i] * 64, h[i] * 64 + 64)
                lw[:, 0 * 4 + d_ * 2 + i, :] = w2[d_][:, ch]
                lw[:, 1 * 4 + d_ * 2 + i, :] = a2[d_][:, ch]
        oh = np.zeros((128, 8), np.float32)
        oh[:, c] = 1.0
        m = {
            "oh": oh, "xs": np.ascontiguousarray(x[c * TOK_PER_CORE:(c + 1) * TOK_PER_CORE]),
            "x": x, "pos": pos, "w1": w1, "w2p": w2p, "w3": w3, "pt": pt, "cst": consts, "amask": amask,
            "gmix": gmix, "gffn": gffn, "gfin": gfin,
            "lw": np.ascontiguousarray(lw.reshape(64, 512)),
            "g2a": np.ascontiguousarray(g2[0:128][:, chs]), "g2b": np.ascontiguousarray(g2[128:160][:, chs]),
            "w_out": w_out, "wr": wr, "br": br,
            "wg": np.ascontiguousarray(wg[4 * c:4 * c + 4]), "wu": np.ascontiguousarray(wu[4 * c:4 * c + 4]),
            "wd": np.ascontiguousarray(wd[4 * c:4 * c + 4]),
        }
        maps.append(m)
    return maps


class KB:
    def __init__(self, nc):
        self.nc = nc
        self.S = Sched(nc)
        self.rr = 0

    def act(self, out, in_, func, bias=None, scale=None, accum=None):
        kw, ins, outs = {}, [in_], [out]
        if bias is not None:
            kw["bias"] = bias
            if _isap(bias):
                ins.append(bias)
        if scale is not None:
            kw["scale"] = scale
            if _isap(scale):
                ins.append(scale)
        if accum is not None:
            kw["accum_out"] = accum
            outs.append(accum)
        return self.S.emit("act", "activation", out, in_, func, ins=ins, outs=outs, **kw)

    def tt(self, eng, out, in0, in1, op):
        return self.S.emit(eng, "tensor_tensor", out, in0, in1, op, ins=[in0, in1], outs=[out])

    def ts(self, eng, out, in0, s1, s2, op0, op1=None):
        ins = [in0] + [s for s in (s1, s2) if _isap(s)]
        if op1 is None:
            return self.S.emit(eng, "tensor_scalar", out, in0, s1, None, op0, ins=ins, outs=[out])
        return self.S.emit(eng, "tensor_scalar", out, in0, s1, s2, op0, op1, ins=ins, outs=[out])

    def stt(self, out, in0, scalar, in1, op0, op1):
        ins = [in0, in1] + ([scalar] if _isap(scalar) else [])
        return self.S.emit("dve", "scalar_tensor_tensor", out, in0, scalar, in1, op0, op1, ins=ins, outs=[out])

    def cp(self, eng, out, in_):
        if eng == "act":
            return self.act(out, in_, AF.Copy)
        return self.S.emit(eng, "tensor_copy", out, in_, ins=[in_], outs=[out])

    def memset(self, eng, out, val):
        return self.S.emit(eng, "memset", out, val, outs=[out])

    def recip(self, out, in_):
        return self.S.emit("dve", "reciprocal", out, in_, ins=[in_], outs=[out])

    def mm(self, out, lhsT, rhs, start=True, stop=True, extra_out=None):
        ins = [lhsT, rhs] + ([] if start else [out])
        outs = [out] if extra_out is None else [extra_out]
        return self.S.emit("pe", "matmul", out, lhsT, rhs, start=start, stop=stop, ins=ins, outs=outs)

    def tr(self, out, in_, ident):
        return self.S.emit("pe", "transpose", out, in_, ident, ins=[in_, ident], outs=[out])

    def scan(self, out, d0, d1, init, op0, op1):
        return self.S.emit("dve", "tensor_tensor_scan", out, d0, d1, init, op0, op1, ins=[d0, d1], outs=[out])

    def rsqrt(self, out, in_, eps):
        self.ts("dve", out, in_, eps, None, ALU.add)
        self.act(out, out, AF.Sqrt)
        self.recip(out, out)


def v3(ap, inner):
    return ap.rearrange("p (a b) -> p a b", b=inner)


def build(upto=99, debug=False, launch=None):
    nc = bass.Bass("TRN2", target_bir_lowering=False)
    K = KB(nc)
    S = K.S
    declared = []

    LUSE = {"x": (1,), "pos": (1,), "w1": (1,), "w2p": (1,), "w3": (1,), "amask": (1,), "lw": (1,), "g2a": (1,), "g2b": (1,),
            "gmix": (1,), "pt": (1,), "cst": (1, 2), "gffn": (2, 3), "gfin": (4,), "w_out": (2,), "oh": (), "xs": (2,),
            "wr": (2,), "br": (2,), "wg": (3,), "wu": (3,), "wd": (3,)}

    def dt_in(name, shape, dt=F32, need=0):
        if upto < need:
            return None
        if launch is not None and launch not in LUSE[name]:
            return None
        declared.append(name)
        return nc.dram_tensor(name, list(shape), dt, kind="ExternalInput").ap()
    x_d = dt_in("x", [SEQ, D])
    pos_d = dt_in("pos", [SEQ], I32)
    w1_d = dt_in("w1", [D, P1_N])
    w2p_d = dt_in("w2p", [D, P2_N], need=2)
    w3_d = dt_in("w3", [D, P3_N], need=3)
    pt_d = dt_in("pt", [128, NPT])
    cst_d = dt_in("cst", [128, NCST])
    amask_d = dt_in("amask", [128, NKB, 512], need=3)
    gmix_d = dt_in("gmix", [128, DC])
    gffn_d = dt_in("gffn", [128, DC], need=4)
    gfin_d = dt_in("gfin", [D], need=6)
    lw_d = dt_in("lw", [64, 512])
    g2a_d = dt_in("g2a", [128, 128])
    g2b_d = dt_in("g2b", [32, 128])
    wout_d = dt_in("w_out", [D, D], need=4)
    oh_d = dt_in("oh", [128, 8], need=4)
    xs_d = dt_in("xs", [TOK_PER_CORE, D], need=4)
    wr_d = dt_in("wr", [D, 36], need=4)
    br_d = dt_in("br", [36], need=4)
    wg_d = dt_in("wg", [4, D, 1024], need=5)
    wu_d = dt_in("wu", [4, D, 1024], need=5)
    wd_d = dt_in("wd", [4, 1024, D], need=5)
    out_d = None
    if launch in (None, 4):
        out_d = nc.dram_tensor("out", [TOK_PER_CORE, D], F32, kind="ExternalOutput").ap()
    ext_in = lambda name, shape, dt=F32: (declared.append(name), nc.dram_tensor(name, list(shape), dt, kind="ExternalInput").ap())[1]
    ext_out = lambda name, shape, dt=F32: nc.dram_tensor(name, list(shape), dt, kind="ExternalOutput").ap()
    L = launch
    dbg = {}
    if debug:
        dbg["yb"] = nc.dram_tensor("dbg_yb", [256, SEQ], BF16, kind="ExternalOutput").ap()
        dbg["yf"] = nc.dram_tensor("dbg_yf", [64, 2, SEQ], F32, kind="ExternalOutput").ap()
        dbg["x2"] = nc.dram_tensor("dbg_x2", [TOK_PER_CORE, D], F32, kind="ExternalOutput").ap()
        dbg["comb"] = nc.dram_tensor("dbg_comb", [TOK_PER_CORE, 32], F32, kind="ExternalOutput").ap()
    yf_t = nc.dram_tensor("yf_scr", [64, 2, SEQ], F32)
    ybounce_t = nc.dram_tensor("ybounce", [256, SEQ], BF16)
    yg_t = nc.dram_tensor("ygath", [NCORE * 256, SEQ], BF16)
    yf_d, ybounce_d, yg_d = yf_t.ap(), ybounce_t.ap(), yg_t.ap()
    if L == 1:
        ybounce_d = ext_out("ybo", [256, SEQ], BF16)
    x2s_t = nc.dram_tensor("x2_scr", [TOK_PER_CORE, D], F32)
    hb_t = nc.dram_tensor("h2bounce", [D, TOK_PER_CORE], BF16)
    hg_t = nc.dram_tensor("h2gath", [NCORE * D, TOK_PER_CORE], BF16)
    cb_t = nc.dram_tensor("cbounce", [TOK_PER_CORE, 32], F32)
    cg_t = nc.dram_tensor("cgath", [SEQ, 32], F32)
    wg16_t = nc.dram_tensor("wg16", [4, 128, DC, 1024], BF16)
    wu16_t = nc.dram_tensor("wu16", [4, 128, DC, 1024], BF16)
    wd16_t = nc.dram_tensor("wd16", [4, 128, 8, D], BF16)
    yp_t = nc.dram_tensor("ypart", [SEQ, D], F32)
    ym_t = nc.dram_tensor("ymoe", [TOK_PER_CORE, D], F32)
    x2s_d, hb_d, hg_d, cb_d, cg_d = x2s_t.ap(), hb_t.ap(), hg_t.ap(), cb_t.ap(), cg_t.ap()
    wg16_d, wu16_d, wd16_d, yp_d, ym_d = wg16_t.ap(), wu16_t.ap(), wd16_t.ap(), yp_t.ap(), ym_t.ap()
    yt_in = cw_in = ypi_d = None
    if L == 2:
        yt_in = ext_in("yt", [D, TOK_PER_CORE], BF16)
        x2s_d = ext_out("x2o", [TOK_PER_CORE, D])
        hb_d = ext_out("hbo", [D, TOK_PER_CORE], BF16)
        cb_d = ext_out("cbo", [TOK_PER_CORE, 32])
    if L == 3:
        hg_d = ext_in("hg", [NCORE * D, TOK_PER_CORE], BF16)
        cw_in = ext_in("cwi", [SEQ, 4])
        yp_d = ext_out("ypo", [SEQ, D])
    if L == 4:
        x2s_d = ext_in("x2i", [TOK_PER_CORE, D])
        ypi_d = ext_in("ypi", [NCORE, TOK_PER_CORE, D])

    st = ExitStack()
    sb = lambda name, shape, dt=F32, stack=None: S.sb(stack or st, name, shape, dt)

    cst = sb("cst_sb", [128, NCST])
    PT = sb("PT", [128, NPT])
    PD = sb("PD", [128, 16])
    identb = sb("identb", [128, 128], BF16)
    rotmb = sb("rotmb", [128, 128], BF16)
    trisb = sb("trisb", [64, 512], BF16)
    triib = sb("triib", [64, 512], BF16)
    tristb = sb("tristb", [64, 512], BF16)
    idrb = sb("idrb", [64, 512], BF16)
    onesb = sb("onesb", [64, 64], BF16)
    meanm = sb("meanm", [64, 64])
    lwb = sb("lwb", [64, 512], BF16)
    g2ab = sb("g2ab", [128, 128], BF16)
    g2bb = sb("g2bb", [32, 128], BF16)
    gmix = sb("gmix_sb", [128, DC])
    with ExitStack() as p0:
      if L in (None, 1, 2):
          lwf = sb("lwf", [64, 512], F32, p0)
          g2af = sb("g2af", [128, 128], F32, p0)
          g2bf = sb("g2bf", [32, 128], F32, p0)
          S.dma("sp", [(cst[:], cst_d)], "c0")
          if L != 2:
              S.dma("sp", [(PT[:], pt_d)], "c1")
              S.dma("sp", [(lwf[:], lw_d), (g2af[:], g2a_d), (g2bf[:], g2b_d), (gmix[:], gmix_d)], "c2")
          else:
              for t_ in (PT, lwf, g2af, g2bf, gmix):
                  K.memset("pool", t_[:], 0.0)
          K.cp("dve", identb[:], cst[:, C_IDENT:C_IDENT + 128])
          K.cp("dve", rotmb[:], cst[:, C_ROTM:C_ROTM + 128])
          K.cp("dve", trisb[:], cst[0:64, C_TRIS:C_TRIS + 512])
          K.cp("dve", triib[:], cst[0:64, C_TRII:C_TRII + 512])
          K.cp("pool", tristb[:], cst[0:64, C_TRIST:C_TRIST + 512])
          K.cp("pool", idrb[:], cst[0:64, C_IDR:C_IDR + 512])
          K.cp("pool", onesb[:], cst[0:64, C_ONES:C_ONES + 64])
          K.ts("pool", meanm[:], cst[0:64, C_ONES:C_ONES + 64], 1.0 / 64.0, None, ALU.mult)
          K.cp("dve", lwb[:], lwf[:])
          K.cp("dve", g2ab[:], g2af[:])
          K.cp("dve", g2bb[:], g2bf[:])
          for gi, g in enumerate(SHIFT_GROUPS):
              K.tt("dve", PD[:, gi:gi + 1], PT[:, PT_IDX["mp_" + g]:PT_IDX["mp_" + g] + 1],
                   PT[:, PT_IDX["mn_" + g]:PT_IDX["mn_" + g] + 1], ALU.add)
          K.ts("dve", PD[:, 0:12], PD[:, 0:12], -1.0, 1.0, ALU.mult, ALU.add)
          K.ts("dve", PD[:, 12:14], PT[:, PT_IDX["ka0"]:PT_IDX["ka0"] + 2], -1.0, 1.0, ALU.mult, ALU.add)
          S.flush()

    def ptc(name, rows=64):
        i = PT_IDX[name]
        return PT[0:rows, i:i + 1]

    def c0c(g):
        i = SHIFT_GROUPS.index(g)
        return PD[0:GW[g], i:i + 1]

    onesf = cst[0:64, C_ONES:C_ONES + 64]
    scanm = cst[0:64, C_SCAN:C_SCAN + 256]
    sel65 = cst[0:65, C_SEL:C_SEL + 64]

    att = ExitStack()
    AT = {}

    def alloc_att():
        AT["qT"] = sb("qT", [128, SEQ], BF16, att)
        AT["kT"] = sb("kT", [128, SEQ], BF16, att)
        AT["Vaug"] = sb("Vaug", [128, 64, 2, 65], BF16, att)

    def run_pass(pidx):
        rev = pidx == 1
        amode = pidx == 2
        rw = not amode
        goff = (P1_OFF, P2_OFF, P3_OFF)[pidx]
        ncols = (P1_N, P2_N, P3_N)[pidx]
        wsrc = (w1_d, w2p_d, w3_d)[pidx]
        dname = "b" if rev else "f"
        if amode:
            qT, kT, Vaug = AT["qT"], AT["kT"], AT["Vaug"]
        with ExitStack() as ps_:
            T = lambda name, shape, dt=F32: S.sb(ps_, "p%d_%s" % (pidx, name), shape, dt)
            wbf = T("wbf", [128, DC, ncols], BF16)
            xt = [T("xt%d" % i, [128, D]) for i in range(2)]
            wst = [xt[0][:, 0:ncols], xt[1][:, 0:ncols]]
            ssq = [T("ssq%d" % i, [128, 1]) for i in range(2)]
            rsx = [T("rsx%d" % i, [128, 1]) for i in range(2)]
            xn = [T("xn%d" % i, [128, D], BF16) for i in range(2)]
            hT = [T("hT%d" % i, [128, DC, TILE + 2], BF16) for i in range(3)]
            TMP1 = T("TMP1", [128, TILE])
            TMP2 = T("TMP2", [128, TILE])
            if rw:
              Rr = T("Rr", [64, 2, TILE])
              Kr = T("Kr", [64, 2, TILE])
              Vr = T("Vr", [64, 2, TILE])
              WLT = T("WLT", [64, TILE], BF16)
              ALB = T("ALB", [64, TILE], BF16)
              SW = T("SW", [64, 2, TILE])
              A_ = T("A_", [64, 2, TILE])
              LD = SW
              KK = T("KK", [64, 2, TILE])
              KK2 = T("KK2", [64, 2, TILE], BF16)
              RIN = T("RIN", [64, 2, TILE])
              KKN = KK
              AN = T("AN", [64, 2, TILE])
              T1 = T("T1", [64, 2, TILE])
              KD = T1
              BV = T("BV", [64, 2, TILE])
              CUM = T("CUM", [64, 2, TILE])
              EP = T("EP", [64, 2, TILE])
              EM = T("EM", [64, 2, TILE])
              DIF = T("DIF", [64, 2, TILE])
              EPP = T("EPP", [64, 2, TILE])
              EH = EPP
              ATRT = T("ATRT", [64, 2, NCHK, 128], BF16)
              BT = T("BT", [64, 2, TILE], BF16)
              KT = T("KT", [64, 2, TILE], BF16)
              BH = T("BH", [64, 2, TILE], BF16)
              KH = T("KH", [64, 2, TILE], BF16)
              VB = T("VB", [64, 2, TILE], BF16)
              VT = T("VT", [64, 8, 64], BF16)
              BHT = T("BHT", [64, 8, 64], BF16)
              KHT = T("KHT", [64, 8, 64], BF16)
              MAB = T("MAB", [64, 8, 64], BF16)
              MABT = T("MABT", [64, 8, 64], BF16)
              MKA = T("MKA", [64, 8, 64], BF16)
              NBR = T("NBR", [64, 8, 64], BF16)
              NKR = T("NKR", [64, 8, 64], BF16)
              Pb = [T("Pb%d" % i, [64, 8, 64], BF16) for i in range(2)]
              PTb = [T("PTb%d" % i, [64, 8, 64], BF16) for i in range(2)]
              Xb = [T("Xb%d" % i, [64, 8, 64], BF16) for i in range(2)]
              W2 = T("W2", [64, 8, 64])
              WS = T("WS", [64, 2, 64], BF16)
              UT16 = T("UT16", [64, 2, 64], BF16)
              ST32 = T("ST32", [64, 2, 64])
              ST16 = T("ST16", [64, 2, 64], BF16)
              YS = T("YS", [64, 2, TILE])
            if rev:
                SGa = T("SGa", [128, TILE], BF16)
                SGb = T("SGb", [32, TILE], BF16)
                YF = T("YF", [64, 2, TILE])
                YC = T("YC", [64, 2, TILE])
                SQ = T("SQ", [64, 2, TILE])
                RS = T("RS", [64, 2, TILE])
                YL = T("YL", [64, 2, TILE])
                RK = T("RK", [64, 2, TILE])
                BON = T("BON", [64, 2, TILE])
                OUTB = T("OUTB", [64, 2, TILE], BF16)
            if amode:
                posi = T("posi", [128, TILE], I32)
                posf = T("posf", [128, TILE])
                ANG = T("ANG", [128, TILE])
                RY = T("RY", [128, TILE])
                KQI = T("KQI", [128, TILE], I32)
                KQF = T("KQF", [128, TILE])
                G1 = T("G1", [128, TILE])
                SINT = T("SINT", [128, TILE])
                COST = T("COST", [128, TILE])
                QF = T("QF", [128, TILE])
                QB = T("QB", [128, TILE], BF16)
                QT1 = T("QT1", [128, TILE])
                QT2 = T("QT2", [128, TILE])
                VBa = T("VBa", [128, TILE], BF16)
            ptr = S.ps(ps_, "p%d_ptr" % pidx, [128, 8, 128], BF16)
            pz = [S.ps(ps_, "p%d_pz%d" % (pidx, i), [128, 512]) for i in range(2)]
            pr = [S.ps(ps_, "p%d_pr%d" % (pidx, i), [128, 512]) for i in range(3)]
            pseq = S.ps(ps_, "p%d_pseq" % pidx, [64, 512])
            pY = S.ps(ps_, "p%d_pY" % pidx, [64, 2, TILE])
            bW1, bUT, bSN = Buf("pseq_w1"), Buf("pseq_ut"), Buf("pseq_sn")
            state = {"bank": 0, "pz": 0}
            DBN = ["Rr", "Kr", "Vr", "ATRT", "W2", "NBR", "NKR", "VT", "KHT", "BHT", "EP"]
            DB = {}
            if rw:
                first = dict(Rr=Rr, Kr=Kr, Vr=Vr, ATRT=ATRT, W2=W2, NBR=NBR, NKR=NKR, VT=VT, KHT=KHT, BHT=BHT, EP=EP)
                for n_ in DBN:
                    t_ = first[n_]
                    DB[n_] = [t_, T(n_ + "_2", list(t_.shape), t_.dtype)]
                DB["Xb"] = [Xb, [T("Xb2_%d" % i_, [64, 8, 64], BF16) for i_ in range(2)]]
                for n_ in ("Rr", "Kr", "Vr"):
                    DB[n_].append(T(n_ + "_3", [64, 2, TILE]))
                DB["WLT"] = [WLT, T("WLT_2", [64, TILE], BF16), T("WLT_3", [64, TILE], BF16)]
                DB["ALB"] = [ALB, T("ALB_2", [64, TILE], BF16), T("ALB_3", [64, TILE], BF16)]
                if rev:
                    DB["SGa"] = [SGa, T("SGa_2", [128, TILE], BF16), T("SGa_3", [128, TILE], BF16)]
                    DB["SGb"] = [SGb, T("SGb_2", [32, TILE], BF16), T("SGb_3", [32, TILE], BF16)]
                    DB["YF"] = [YF, T("YF_2", [64, 2, TILE]), T("YF_3", [64, 2, TILE])]

            def bank():
                b = pr[state["bank"] % (3 if amode else 2)]
                state["bank"] += 1
                return b

            def tbank():
                return pr[2]

            def pzbank():
                b = pz[state["pz"] % 2]
                state["pz"] += 1
                return b

            for dc in range(DC):
                ws = wst[dc % 2]
                S.dma("sp", [(ws, wsrc[dc * 128:(dc + 1) * 128, :])], "w%d" % (dc % 2))
                K.ts("dve" if dc % 2 == 0 else "pool", wbf[:, dc, :], ws, gmix[:, dc:dc + 1], None, ALU.mult)
            if rw:
                K.memset("dve", ST32[:], 0.0)
                K.memset("dve", ST16[:], 0.0)
            if amode:
                K.memset("pool", Vaug[:, :, :, 64:65], 1.0)

            def prep_tile(i):
                slot = i % 3
                for blk in range(2):
                    tb = 2 * i + blk
                    xs = xt[blk]
                    S.dma("sp", [(xs[:], x_d[tb * 128:(tb + 1) * 128, :])], "x%d" % blk)
                    K.act(xn[blk][:], xs[:], AF.Square, accum=ssq[blk][:])
                    K.ts("dve", rsx[blk][:], ssq[blk][:], 1.0 / D, 1e-6, ALU.mult, ALU.add)
                    K.act(rsx[blk][:], rsx[blk][:], AF.Sqrt)
                    K.recip(rsx[blk][:], rsx[blk][:])
                    if blk == 0:
                        K.act(xn[blk][:], xs[:], AF.Copy, scale=rsx[blk][:])
                    else:
                        K.ts("dve", xn[blk][:], xs[:], rsx[blk][:], None, ALU.mult)
                    for half in range(2):
                        for j in range(8):
                            dc = half * 8 + j
                            K.tr(ptr[:, j, :], xn[blk][:, dc * 128:(dc + 1) * 128], identb[:])
                        dst = hT[slot][:, half * 8:(half + 1) * 8, 1 + blk * 128:1 + blk * 128 + 128]
                        if half == 0:
                            K.cp("act", dst, ptr[:])
                        else:
                            K.cp("dve", dst, ptr[:])

            def shift(g, pzb, dst, eng2="dve", final_func=None):
                M = GW[g]
                mp = PT[0:M, PT_IDX["mp_" + g]:PT_IDX["mp_" + g] + 1]
                mn = PT[0:M, PT_IDX["mn_" + g]:PT_IDX["mn_" + g] + 1]
                K.act(TMP1[0:M, :], pzb[0:M, 1:257], AF.Copy, scale=c0c(g))
                K.stt(TMP2[0:M, :], pzb[0:M, 0:256], mp, TMP1[0:M, :], ALU.mult, ALU.add)
                if final_func is None:
                    K.stt(dst, pzb[0:M, 2:258], mn, TMP2[0:M, :], ALU.mult, ALU.add)
                else:
                    K.stt(TMP1[0:M, :], pzb[0:M, 2:258], mn, TMP2[0:M, :], ALU.mult, ALU.add)
                    K.act(dst, TMP1[0:M, :], final_func)

            def R_(ap):
                if not rev:
                    return ap
                idx = (slice(None),) * (len(ap.shape) - 1) + (slice(None, None, -1),)
                return ap[idx]

            def process_tile(i, jj):
                par, tri = jj % 2, jj % 3
                slot = i % 3
                t0 = i * TILE
                h = hT[slot]
                if rw:
                    Rr, Kr, Vr, ATRT, W2, NBR, NKR, VT, KHT, BHT, EP = (DB[n_][par] for n_ in DBN)
                    Rr, Kr, Vr, WLT, ALB = (DB[n_][tri] for n_ in ("Rr", "Kr", "Vr", "WLT", "ALB"))
                    Xb = DB["Xb"][par]
                if rev:
                    SGa, SGb, YF = DB["SGa"][tri], DB["SGb"][tri], DB["YF"][tri]
                if i > 0:
                    K.cp("act", h[:, :, 0:1], hT[(i - 1) % 3][:, :, TILE:TILE + 1])
                else:
                    K.memset("dve", h[:, :, 0:1], 0.0)
                if i < NT - 1:
                    K.cp("act", h[:, :, TILE + 1:TILE + 2], hT[(i + 1) % 3][:, :, 1:2])
                else:
                    K.memset("dve", h[:, :, TILE + 1:TILE + 2], 0.0)
                if rev:
                    S.dma("sp", [(YF[:], yf_d[:, :, t0:t0 + TILE])], "yfl")

                def inproj(g):
                    M = GW[g]
                    pzb = pzbank()
                    o = goff[g]
                    for dc in range(DC):
                        K.mm(pzb[0:M, 0:TILE + 2], wbf[:, dc, o:o + M], h[:, dc, :], start=(dc == 0), stop=(dc == DC - 1))
                    return pzb

                if rw:
                    for hh in range(2):
                        shift("r%d" % hh, inproj("r%d" % hh), R_(Rr[:, hh, :]))
                        shift("k%d" % hh, inproj("k%d" % hh), R_(Kr[:, hh, :]))
                        shift("v%d" % hh, inproj("v%d" % hh), R_(Vr[:, hh, :]))
                    shift("wl" + dname, inproj("wl" + dname), R_(WLT[:]), final_func=AF.Tanh)
                    shift("al" + dname, inproj("al" + dname), R_(ALB[:]))
                if rev:
                    shift("gla", inproj("gla"), R_(SGa[:]), final_func=AF.Sigmoid)
                    shift("glb", inproj("glb"), R_(SGb[:]), final_func=AF.Sigmoid)
                if amode:
                    S.dma("sp", [(posi[:], pos_d[t0:t0 + TILE].partition_broadcast(128))], "pos")
                    K.cp("pool", posf[:], posi[:])
                    K.ts("pool", ANG[:], posf[:], ptc("invf", 128), None, ALU.mult)
                    for (tab, shiftv) in ((SINT, 0.0), (COST, float(np.pi / 2))):
                        K.ts("pool", RY[:], ANG[:], shiftv, None, ALU.add)
                        K.ts("pool", KQI[:], RY[:], float(1.0 / (2 * np.pi)), None, ALU.mult)
                        K.cp("pool", KQF[:], KQI[:])
                        K.ts("pool", KQF[:], KQF[:], float(-2 * np.pi), None, ALU.mult)
                        K.tt("pool", RY[:], RY[:], KQF[:], ALU.add)
                        K.ts("pool", G1[:], RY[:], float(np.pi), float(-2 * np.pi), ALU.is_gt, ALU.mult)
                        K.tt("pool", RY[:], RY[:], G1[:], ALU.add)
                        K.ts("pool", G1[:], RY[:], float(-np.pi), float(2 * np.pi), ALU.is_lt, ALU.mult)
                        K.tt("pool", RY[:], RY[:], G1[:], ALU.add)
                        K.ts("pool", RY[:], RY[:], float(np.pi), float(-np.pi), ALU.min, ALU.max)
                        K.act(tab[:], RY[:], AF.Sin)
                    for (g, dstT) in (("aq", qT), ("ak", kT)):
                        pzb = inproj(g)
                        K.act(QF[:], pzb[:, 1:TILE + 1], AF.Copy)
                        K.cp("dve", QB[:], pzb[:, 1:TILE + 1])
                        pb = bank()
                        K.mm(pb[:, 0:TILE], rotmb[:], QB[:])
                        K.tt("pool", QT1[:], QF[:], COST[:], ALU.mult)
                        K.tt("dve", QT2[:], pb[:, 0:TILE], SINT[:], ALU.mult)
                        K.tt("pool", dstT[:, t0:t0 + TILE], QT1[:], QT2[:], ALU.add)
                    pzb = inproj("av")
                    K.act(VBa[:], pzb[:, 1:TILE + 1], AF.Copy)
                    pb = bank()
                    pbv = pb[:, 0:128].bitcast(BF16).rearrange("p (b c) -> p b c", c=128)
                    for blk in range(2):
                        K.tr(pbv[:, blk, :], VBa[:, blk * 128:(blk + 1) * 128], identb[:])
                    K.cp("dve", Vaug[:, 2 * i:2 * i + 2, :, 0:64],
                         pbv.rearrange("p b (h d) -> p b h d", d=64))
                    return

                S.seg = segs["A2"]
                d_ = 1 if rev else 0
                pw_, pa_ = bank(), bank()
                pwv, pav = v3(pw_[0:64, :], TILE), v3(pa_[0:64, :], TILE)
                for hh in range(2):
                    K.mm(pwv[:, hh, :], lwb[:, (0 * 4 + d_ * 2 + hh) * 64:(0 * 4 + d_ * 2 + hh) * 64 + 64], WLT[:])
                    K.mm(pav[:, hh, :], lwb[:, (1 * 4 + d_ * 2 + hh) * 64:(1 * 4 + d_ * 2 + hh) * 64 + 64], ALB[:])
                for hh in range(2):
                    K.act(SW[:, hh, :], pwv[:, hh, :], AF.Sigmoid, bias=ptc("w0%s%d" % (dname, hh)))
                    K.act(A_[:, hh, :], pav[:, hh, :], AF.Sigmoid, bias=ptc("a0%s%d" % (dname, hh)))
                K.ts("dve", LD[:], SW[:], -0.6065306597126334, None, ALU.mult)
                for hh in range(2):
                    K.ts("dve", KK[:, hh, :], Kr[:, hh, :], ptc("kk%d" % hh), None, ALU.mult)
                K.tt("dve", KK2[:], KK[:], KK[:], ALU.mult)
                pq_ = bank()
                pqv = v3(pq_[0:64, :], TILE)
                for hh in range(2):
                    K.mm(pqv[:, hh, :], onesb[:], KK2[:, hh, :])
                K.rsqrt(RIN[:], pqv, 1e-12)
                K.tt("dve", KKN[:], KK[:], RIN[:], ALU.mult)
                K.ts("dve", AN[:], KKN[:], -1.0, None, ALU.mult)
                for hh in range(2):
                    K.ts("dve", T1[:, hh, :], A_[:, hh, :], ptc("ka%d" % hh), PD[0:64, 12 + hh:13 + hh], ALU.mult, ALU.add)
                K.tt("dve", KD[:], Kr[:], T1[:], ALU.mult)
                K.tt("dve", BV[:], KKN[:], A_[:], ALU.mult)
                for hh in range(2):
                    K.scan(CUM[:, hh, :], scanm, LD[:, hh, :], 0.0, ALU.mult, ALU.add)
                K.act(EP[:], CUM[:], AF.Exp)
                K.act(EM[:], CUM[:], AF.Exp, scale=-1.0)
                K.tt("dve", DIF[:], CUM[:], LD[:], ALU.subtract)
                K.act(EPP[:], DIF[:], AF.Exp)
                c4 = lambda ap: ap.rearrange("p h (c t) -> p h c t", t=CH)
                K.tt("dve", ATRT[:, :, :, 0:64], c4(AN[:]), c4(EPP[:]), ALU.mult)
                K.tt("dve", ATRT[:, :, :, 64:128], c4(Rr[:]), c4(EP[:]), ALU.mult)
                K.tt("dve", BT[:], BV[:], EM[:], ALU.mult)
                K.tt("dve", KT[:], KD[:], EM[:], ALU.mult)
                for hh in range(2):
                    for c in range(NCHK):
                        K.ts("dve", DIF[:, hh, c * CH:(c + 1) * CH], CUM[:, hh, c * CH:(c + 1) * CH],
                             -1.0, CUM[:, hh, c * CH + CH - 1:c * CH + CH], ALU.mult, ALU.add)
                K.act(EH[:], DIF[:], AF.Exp)
                K.tt("dve", BH[:], BV[:], EH[:], ALU.mult)
                K.tt("dve", KH[:], KD[:], EH[:], ALU.mult)
                K.cp("act", VB[:], Vr[:])
                for (src, dstT_) in ((VB, VT), (BH, BHT), (KH, KHT)):
                    pb = bank()
                    pbv = pb[0:64, 0:256].bitcast(BF16).rearrange("p (a b) -> p a b", b=64)
                    for c in range(NCHK):
                        for hh in range(2):
                            K.tr(pbv[:, c * 2 + hh, :], src[:, hh, c * CH:(c + 1) * CH], identb[0:64, 0:64])
                    K.cp("act", dstT_[:], pbv)
                for (lt, dm, dn) in ((BT, MAB, NBR), (KT, MKA, NKR)):
                    for half in range(2):
                        pb = bank()
                        pbv = v3(pb[0:64, :], 128)
                        for j in range(4):
                            idx = half * 4 + j
                            c, hh = idx // 2, idx % 2
                            K.mm(pbv[:, j, :], lt[:, hh, c * CH:(c + 1) * CH], ATRT[:, hh, c, :])
                        K.tt("dve", dm[:, half * 4:half * 4 + 4, :], pbv[:, :, 0:64], v3(trisb[:, 0:256], 64), ALU.mult)
                        K.tt("dve", dn[:, half * 4:half * 4 + 4, :], pbv[:, :, 64:128], v3(triib[:, 0:256], 64), ALU.mult)
                pb = bank()
                pbv = v3(pb[0:64, :], 64)
                for idx in range(8):
                    c, hh = idx // 2, idx % 2
                    K.mm(pbv[:, idx, :], ATRT[:, hh, c, 0:64], BT[:, hh, c * CH:(c + 1) * CH])
                K.tt("dve", MABT[:], pbv, v3(tristb[:], 64), ALU.mult)
                P_, PT_, X_ = MAB, MABT, Xb[0]
                K.tt("dve", X_[:], MAB[:], v3(idrb[:], 64), ALU.add)
                for lvl in range(5):
                    last = lvl == 4
                    PTn, Pn, Xn = PTb[lvl % 2], Pb[lvl % 2], Xb[(lvl + 1) % 2]
                    pb1 = bank()
                    pv1 = v3(pb1[0:64, :], 64)
                    for idx in range(8):
                        K.mm(pv1[:, idx, :], P_[:, idx, :], PT_[:, idx, :])
                    K.cp("act", PTn[:], pv1)
                    if not last:
                        pb2 = bank()
                        pv2 = v3(pb2[0:64, :], 64)
                        for idx in range(8):
                            K.mm(pv2[:, idx, :], PT_[:, idx, :], P_[:, idx, :])
                        K.cp("dve", Pn[:], pv2)
                    pb3 = bank()
                    pv3 = v3(pb3[0:64, :], 64)
                    for idx in range(8):
                        K.mm(pv3[:, idx, :], PTn[:, idx, :], X_[:, idx, :])
                    K.tt("dve", Xn[:], pv3, X_[:], ALU.add)
                    P_, PT_, X_ = Pn, PTn, Xn
                Tm = X_
                pb = bank()
                pbv = v3(pb[0:64, :], 64)
                for idx in range(8):
                    K.mm(pbv[:, idx, :], MKA[:, idx, :], VT[:, idx, :])
                K.cp("act", W2[:], pbv)
                S.seg = segs["B"]
                sW1 = v3(pseq[:, 0:128], 64)
                sUT = v3(pseq[:, 128:256], 64)
                sSN = v3(pseq[:, 256:384], 64)
                for c in range(NCHK):
                    cs = slice(c * CH, (c + 1) * CH)
                    for hh in range(2):
                        S.emit("pe", "matmul", sW1[:, hh, :], ATRT[:, hh, c, 0:64], ST16[:, hh, :], start=True, stop=True,
                               ins=[ATRT[:], ST16[:]], outs=[bW1])
                    S.emit("dve", "tensor_tensor", WS[:], sW1, W2[:, 2 * c:2 * c + 2, :], ALU.add,
                           ins=[bW1, W2[:]], outs=[WS[:]])
                    for hh in range(2):
                        S.emit("pe", "matmul", sUT[:, hh, :], Tm[:, 2 * c + hh, :], WS[:, hh, :], start=True, stop=True,
                               ins=[Tm[:], WS[:]], outs=[bUT])
                    S.emit("act", "activation", UT16[:], sUT, AF.Copy, ins=[bUT], outs=[UT16[:]])
                    for hh in range(2):
                        K.mm(pY[:, hh, cs], ST16[:, hh, :], ATRT[:, hh, c, 64:128], start=True, stop=False)
                        K.mm(pY[:, hh, cs], VT[:, 2 * c + hh, :], NKR[:, 2 * c + hh, :], start=False, stop=False)
                        K.mm(pY[:, hh, cs], UT16[:, hh, :], NBR[:, 2 * c + hh, :], start=False, stop=True)
                    for hh in range(2):
                        S.emit("pe", "matmul", sSN[:, hh, :], KHT[:, 2 * c + hh, :], VT[:, 2 * c + hh, :], start=True, stop=False,
                               ins=[KHT[:], VT[:]], outs=[bSN])
                        S.emit("pe", "matmul", sSN[:, hh, :], BHT[:, 2 * c + hh, :], UT16[:, hh, :], start=False, stop=True,
                               ins=[BHT[:], UT16[:], bSN], outs=[bSN])
                    for hh in range(2):
                        S.emit("dve", "scalar_tensor_tensor", ST32[:, hh, :], ST32[:, hh, :],
                               EP[:, hh, c * CH + CH - 1:c * CH + CH], sSN[:, hh, :], ALU.mult, ALU.add,
                               ins=[ST32[:], EP[:], bSN], outs=[ST32[:]])
                    K.cp("act", ST16[:], ST32[:])
                if not rev:
                    K.cp("act", YS[:], pY[:])
                    S.dma("sp", [(yf_d[:, :, t0:t0 + TILE], YS[:])], "yfs")
                else:
                    K.tt("dve", YS[:], pY[:], YF[:, :, ::-1], ALU.add)
                    flat = lambda ap: ap.rearrange("p h t -> p (h t)")
                    pm_ = tbank()
                    K.mm(pm_[0:64, :], meanm[:], flat(YS[:]))
                    K.tt("dve", flat(YC[:]), flat(YS[:]), pm_[0:64, :], ALU.subtract)
                    K.tt("dve", SQ[:], YC[:], YC[:], ALU.mult)
                    pv_ = tbank()
                    K.mm(pv_[0:64, :], meanm[:], flat(SQ[:]))
                    K.rsqrt(flat(RS[:]), pv_[0:64, :], 64e-5)
                    K.tt("dve", YC[:], YC[:], RS[:], ALU.mult)
                    for hh in range(2):
                        K.ts("dve", YL[:, hh, :], YC[:, hh, :], ptc("lnw%d" % hh), ptc("lnb%d" % hh), ALU.mult, ALU.add)
                    K.tt("dve", RK[:], Rr[:], Kr[:], ALU.mult)
                    for hh in range(2):
                        K.ts("dve", RK[:, hh, :], RK[:, hh, :], ptc("rk%d" % hh), None, ALU.mult)
                    pb_ = tbank()
                    K.mm(pb_[0:64, :], onesf, flat(RK[:]))
                    K.tt("dve", flat(BON[:]), pb_[0:64, :], flat(Vr[:]), ALU.mult)
                    pg_ = tbank()
                    pgv = v3(pg_[0:64, :], TILE)
                    for hh in range(2):
                        K.mm(pgv[:, hh, :], g2ab[:, hh * 64:(hh + 1) * 64], SGa[:], start=True, stop=False)
                        K.mm(pgv[:, hh, :], g2bb[:, hh * 64:(hh + 1) * 64], SGb[:], start=False, stop=True)
                    K.tt("dve", YL[:], YL[:], BON[:], ALU.add)
                    K.tt("dve", OUTB[:, :, ::-1], YL[:], pgv, ALU.mult)
                    S.dma("sp", [(ybounce_d[0:128, t0:t0 + TILE].rearrange("(h c) t -> c h t", c=64), OUTB[:])], "ybs")

            order = list(range(NT - 1, -1, -1)) if rev else list(range(NT))
            segs = {}

            REC = {}

            def record(j):
                if j >= NT:
                    return [], [], []
                if j not in REC:
                    segs["A1"], segs["A2"], segs["B"] = [], [], []
                    S.seg = segs["A1"]
                    process_tile(order[j], j)
                    S.seg = None
                    REC[j] = (segs["A1"], segs["A2"], segs["B"])
                return REC[j]

            prep_tile(order[0])
            prep_tile(order[1])
            S.commit_merged(record(0)[0])
            prep_tile(order[2])
            S.commit_merged(record(0)[1], record(1)[0])
            for t in range(NT):
                if t + 3 < NT:
                    prep_tile(order[t + 3])
                S.commit_merged(record(t)[2], record(t + 1)[1], record(t + 2)[0])
                REC.pop(t, None)
            S.flush()

    def attention_phase():
        alloc_att()
        run_pass(2)
        qT, kT, Vaug = AT["qT"], AT["kT"], AT["Vaug"]
        with ExitStack() as a_:
            T = lambda name, shape, dt=F32: S.sb(a_, "at_" + name, shape, dt)
            MASK = T("MASK", [128, NKB, 512], BF16)
            mst = [T("mst%d" % i, [128, 512]) for i in range(2)]
            E = [T("E%d" % i, [128, 512], BF16) for i in range(3)]
            EMk = [T("EMk%d" % i, [128, 512], BF16) for i in range(3)]
            OTs = T("OTs", [65, 512])
            RC = T("RC", [64, 512])
            YA = [T("YA%d" % i, [64, 512], BF16) for i in range(2)]
            pS = [S.ps(a_, "at_pS%d" % i, [128, 512]) for i in range(3)]
            pO = [S.ps(a_, "at_pO%d" % i, [128, 512]) for i in range(2)]
            pD = S.ps(a_, "at_pD", [64, 512])
            for off in range(NKB):
                S.dma("sp", [(mst[off % 2][:], amask_d[:, off, :])], "am%d" % (off % 2))
                K.cp("dve" if off % 2 == 0 else "pool", MASK[:, off, :], mst[off % 2][:])
            it = 0
            n_ = 0
            for hh in range(2):
                hs = slice(hh * 64, (hh + 1) * 64)
                for qt in range(SEQ // 512):
                    q0 = qt * 512
                    kbs = [kb for kb in range(q0 // 128 - 8, q0 // 128 + 12) if 0 <= kb < SEQ // 128]
                    pO_ = pO[it % 2]
                    for n, kb in enumerate(kbs):
                        off = kb - q0 // 128 + 8
                        pS_ = pS[n_ % 3]
                        E_ = E[n_ % 3]
                        EM_ = EMk[n_ % 3]
                        K.mm(pS_[:], kT[hs, kb * 128:(kb + 1) * 128], qT[hs, q0:q0 + 512])
                        K.act(E_[:], pS_[:], AF.Exp, scale=0.125)
                        K.tt("dve" if n_ % 2 == 0 else "pool", EM_[:], E_[:], MASK[:, off, :], ALU.mult)
                        K.mm(pO_[0:65, :], Vaug[:, kb, hh, :], EM_[:], start=(n == 0), stop=(n == len(kbs) - 1))
                        n_ += 1
                    K.cp("act", OTs[:], pO_[0:65, :])
                    K.mm(pD[:], sel65, OTs[:])
                    K.recip(RC[:], pD[:])
                    ya = YA[it % 2]
                    K.tt("dve", ya[:], OTs[0:64, :], RC[:], ALU.mult)
                    S.dma("sp", [(ybounce_d[128 + hh * 64:128 + hh * 64 + 64, q0:q0 + 512], ya[:])], "yas")
                    it += 1
            if L is None:
                S.collective("AllGather", ALU.bypass, ybounce_t, yg_t, "ag1")
            S.flush()
        att.close()

    AXX = mybir.AxisListType.X

    def phase4():
        OH = sb("OH", [128, 8])
        gffn = sb("gffn_sb", [128, DC])
        if L is None:
            S.dma("sp", [(OH[:], oh_d), (gffn[:], gffn_d)], "p4c")
        else:
            S.dma("sp", [(gffn[:], gffn_d)], "p4c")
        with ExitStack() as p4:
            YTs = S.sb(p4, "YTs", [128, DC, TOK_PER_CORE], BF16)
            with ExitStack() as p4a:
                if L is not None:
                    S.dma("sp", [(YTs[:], yt_in.rearrange("(kc p) t -> p kc t", p=128))], "ytl")
                blk = [S.sb(p4a, "yblk%d" % i, [128, DC, TOK_PER_CORE], BF16) for i in range(2)] if L is None else []
                for j in range(NCORE if L is None else 0):
                    b = blk[j % 2]
                    S.dma("sp", [(b[:], yg_d[:, j * TOK_PER_CORE:(j + 1) * TOK_PER_CORE].rearrange("(kc p) t -> p kc t", p=128))],
                          "yb%d" % (j % 2))
                    for half in range(2):
                        hs = slice(half * 8, half * 8 + 8)
                        if j == 0:
                            K.ts("dve", YTs[:, hs, :], b[:, hs, :], OH[:, 0:1], None, ALU.mult)
                        else:
                            K.stt(YTs[:, hs, :], b[:, hs, :], OH[:, j:j + 1], YTs[:, hs, :], ALU.mult, ALU.add)
                S.flush()
            import os
            P4STOP = int(os.environ.get("P4STOP", "9"))
            if P4STOP <= 1:
                return OH, gffn
            T = lambda name, shape, dt=F32: S.sb(p4, "p4_" + name, shape, dt)
            WOb = T("WOb", [128, DC, D], BF16)
            WRf = T("WRf", [128, DC, 36])
            BR = T("BR", [128, 36])
            xtl = [T("xt%d" % i, [128, D]) for i in range(2)]
            wos = xtl
            X2 = [T("X2_%d" % i, [128, D]) for i in range(2)]
            junk = T("junk", [128, D], BF16)
            H2f = T("H2f", [128, D])
            Hhi = T("Hhi", [128, D], BF16)
            Hlo = T("Hlo", [128, D], BF16)
            h2Tl = T("h2Tl", [128, DC, 128], BF16)
            WRhi = T("WRhi", [128, DC, 36], BF16)
            WRlo = T("WRlo", [128, DC, 36], BF16)
            h2Tb = T("h2Tb", [128, DC, TOK_PER_CORE], BF16)
            sm = lambda name, n: T(name, [128, n])
            ssq, rstd = sm("ssq", 1), sm("rstd", 1)
            LG, gmax, ngmax, GOH, EG, gsum, g1 = sm("L", 36), sm("gmax", 1), sm("ngmax", 1), sm("GOH", 4), sm("EG", 4), sm("gsum", 1), sm("g1", 1)
            el, m1, OH1, el2, m2, OH2 = sm("el", 8), sm("m1", 1), sm("OH1", 8), sm("el2", 8), sm("m2", 1), sm("OH2", 8)
            dm_, e2, den, w1, w2_, W12, WGt = sm("dm", 1), sm("e2", 1), sm("den", 1), sm("w1", 1), sm("w2", 1), sm("W12", 8), sm("WGt", 8)
            COMB = [T("COMB%d" % i, [128, 32]) for i in range(2)]
            pp = [S.ps(p4, "p4_pp%d" % i, [128, 512]) for i in range(4)]
            ptp = [S.ps(p4, "p4_pt%d" % i, [128, 8, 128], BF16) for i in range(2)]
            pl = S.ps(p4, "p4_pl", [128, 64])
            identf = cst[:, C_IDENT:C_IDENT + 128]
            for kc in range(DC):
                r0 = (kc % 2) * 1024 + (kc // 2) * 128
                ws = wos[kc % 2]
                S.dma("sp", [(ws[:], wout_d[r0:r0 + 128, :])], "wo%d" % (kc % 2))
                K.cp("dve" if kc % 2 == 0 else "pool", WOb[:, kc, :], ws[:])
            S.dma("sp", [(WRf[:], wr_d.rearrange("(dc p) n -> p dc n", p=128)), (BR[:], br_d.partition_broadcast(128))], "wr")
            for dc in range(DC):
                K.ts("dve", WRf[:, dc, :], WRf[:, dc, :], gffn[:, dc:dc + 1], None, ALU.mult)
            K.cp("dve", WRhi[:], WRf[:])
            K.tt("dve", WRlo[:], WRf[:], WRhi[:], ALU.subtract)
            nb = 0
            if P4STOP <= 2:
                S.flush()
                return OH, gffn
            for tb in range(TOK_PER_CORE // 128):
                xt_ = xtl[tb % 2]
                x2 = X2[tb % 2]
                S.dma("sp", [(xt_[:], xs_d[tb * 128:(tb + 1) * 128, :])], "xs%d" % (tb % 2))
                for n4 in range(4):
                    pb = pp[nb % 4]
                    nb += 1
                    for kc in range(DC):
                        K.mm(pb[:], YTs[:, kc, tb * 128:(tb + 1) * 128], WOb[:, kc, n4 * 512:(n4 + 1) * 512],
                             start=(kc == 0), stop=(kc == DC - 1))
                    K.tt("dve", x2[:, n4 * 512:(n4 + 1) * 512], pb[:], xt_[:, n4 * 512:(n4 + 1) * 512], ALU.add)
                S.dma("sp", [(x2s_d[tb * 128:(tb + 1) * 128, :], x2[:])], "x2s")
                if debug:
                    S.dma("sp", [(dbg["x2"][tb * 128:(tb + 1) * 128, :], x2[:])], "x2d")
                if P4STOP <= 3:
                    continue
                K.act(junk[:], x2[:], AF.Square, accum=ssq[:])
                K.ts("dve", rstd[:], ssq[:], 1.0 / D, 1e-6, ALU.mult, ALU.add)
                K.act(rstd[:], rstd[:], AF.Sqrt)
                K.recip(rstd[:], rstd[:])
                K.act(H2f[:], x2[:], AF.Copy, scale=rstd[:])
                K.cp("act", Hhi[:], H2f[:])
                K.tt("dve", Hlo[:], H2f[:], Hhi[:], ALU.subtract)
                tcols = slice(tb * 128, (tb + 1) * 128)
                nq = 0
                for (src, dstf) in ((Hhi, lambda h8: h2Tb[:, h8 * 8:h8 * 8 + 8, tcols]), (Hlo, lambda h8: h2Tl[:, h8 * 8:h8 * 8 + 8, :])):
                    for h8 in range(2):
                        pt_ = ptp[nq % 2]
                        nq += 1
                        for j in range(8):
                            dc = h8 * 8 + j
                            K.tr(pt_[:, j, :], src[:, dc * 128:(dc + 1) * 128], identb[:])
                        K.cp("act" if h8 == 0 else "dve", dstf(h8), pt_[:])
                if P4STOP <= 4:
                    continue
                nmm = 0
                for dc in range(DC):
                    for (lt, rt) in ((h2Tb[:, dc, tcols], WRhi), (h2Tb[:, dc, tcols], WRlo), (h2Tl[:, dc, :], WRhi)):
                        K.mm(pl[:, 0:36], lt, rt[:, dc, :], start=(nmm == 0), stop=(nmm == 3 * DC - 1))
                        nmm += 1
                K.tt("dve", LG[:], pl[:, 0:36], BR[:], ALU.add)
                red = lambda out, in_: S.emit("dve", "reduce_max", out, in_, AXX, ins=[in_], outs=[out])
                red(gmax[:], LG[:, 0:4])
                K.ts("dve", GOH[:], LG[:, 0:4], gmax[:], None, ALU.is_equal)
                K.ts("dve", ngmax[:], gmax[:], -1.0, None, ALU.mult)
                K.act(EG[:], LG[:, 0:4], AF.Exp, bias=ngmax[:], accum=gsum[:])
                K.recip(g1[:], gsum[:])
                K.ts("dve", el[:], LG[:, 4:12], GOH[:, 0:1], None, ALU.mult)
                for g in range(1, 4):
                    K.stt(el[:], LG[:, 4 + 8 * g:12 + 8 * g], GOH[:, g:g + 1], el[:], ALU.mult, ALU.add)
                red(m1[:], el[:])
                K.ts("dve", OH1[:], el[:], m1[:], None, ALU.is_equal)
                K.stt(el2[:], OH1[:], -1e30, el[:], ALU.mult, ALU.add)
                red(m2[:], el2[:])
                K.ts("dve", OH2[:], el2[:], m2[:], None, ALU.is_equal)
                K.tt("dve", dm_[:], m2[:], m1[:], ALU.subtract)
                K.act(e2[:], dm_[:], AF.Exp)
                K.ts("dve", den[:], e2[:], 1.0, None, ALU.add)
                K.recip(w1[:], den[:])
                K.tt("dve", w2_[:], e2[:], w1[:], ALU.mult)
                K.ts("dve", W12[:], OH1[:], w1[:], None, ALU.mult)
                K.stt(W12[:], OH2[:], w2_[:], W12[:], ALU.mult, ALU.add)
                K.ts("dve", WGt[:], W12[:], g1[:], None, ALU.mult)
                cm = COMB[tb % 2]
                for g in range(4):
                    K.ts("dve", cm[:, g * 8:(g + 1) * 8], WGt[:], GOH[:, g:g + 1], None, ALU.mult)
                S.dma("sp", [(cb_d[tb * 128:(tb + 1) * 128, :], cm[:])], "cbs")
                if debug:
                    S.dma("sp", [(dbg["comb"][tb * 128:(tb + 1) * 128, :], cm[:])], "cbd")
            S.dma("sp", [(hb_d.rearrange("(dc p) t -> p dc t", p=128), h2Tb[:])], "hbs")
            import os
            if L is None and not os.environ.get("DBG_NOCC2"):
                S.collective("AllGather", ALU.bypass, hb_t, hg_t, "ag2")
                S.collective("AllGather", ALU.bypass, cb_t, cg_t, "ag3")
            S.flush()
        return OH, gffn

    def phase5(OH, gffn):
        with ExitStack() as p5:
            T = lambda name, shape, dt=F32: S.sb(p5, "p5_" + name, shape, dt)
            with ExitStack() as p5a:
                st_ = [S.sb(p5a, "p5_wst%d" % i, [128, D], F32) for i in range(3)]
                ob_ = [S.sb(p5a, "p5_wob%d" % i, [128, D], BF16) for i in range(3)]
                n = 0
                for (src, dst) in ((wg_d, wg16_d), (wu_d, wu16_d)):
                    for e in range(4):
                        for dc in range(0, DC, 2):
                            s_, o_ = st_[n % 3], ob_[n % 3]
                            S.dma("sp", [(s_[:, 0:1024], src[e, dc * 128:(dc + 1) * 128, :]),
                                         (s_[:, 1024:2048], src[e, (dc + 1) * 128:(dc + 2) * 128, :])], "ws%d" % (n % 3))
                            for k2 in range(2):
                                if (n + k2) % 2 == 0:
                                    K.ts("dve", o_[:, k2 * 1024:(k2 + 1) * 1024], s_[:, k2 * 1024:(k2 + 1) * 1024],
                                         gffn[:, dc + k2:dc + k2 + 1], None, ALU.mult)
                                else:
                                    K.act(o_[:, k2 * 1024:(k2 + 1) * 1024], s_[:, k2 * 1024:(k2 + 1) * 1024], AF.Copy,
                                          scale=gffn[:, dc + k2:dc + k2 + 1])
                            S.dma("sp", [(dst[e, :, dc:dc + 2, :], o_[:].rearrange("p (a b) -> p a b", b=1024))], "wt%d" % (n % 3))
                            n += 1
                for e in range(4):
                    for hc in range(8):
                        s_, o_ = st_[n % 3], ob_[n % 3]
                        S.dma("sp", [(s_[:], wd_d[e, hc * 128:(hc + 1) * 128, :])], "ws%d" % (n % 3))
                        K.cp(("dve", "act")[n % 2], o_[:], s_[:])
                        S.dma("sp", [(wd16_d[e, :, hc, :], o_[:])], "wt%d" % (n % 3))
                        n += 1
                S.flush()
            CW = T("CW", [128, SEQ // 128, 4])
            if L is None:
                CG = T("CG", [128, SEQ // 128, 32])
                S.dma("sp", [(CG[:], cg_d.rearrange("(tb p) e -> p tb e", p=128))], "cgl")
            else:
                S.dma("sp", [(CW[:], cw_in.rearrange("(tb p) e -> p tb e", p=128))], "cgl")
            for c in range(NCORE if L is None else 0):
                if c == 0:
                    K.ts("dve", CW[:], CG[:, :, 0:4], OH[:, 0:1], None, ALU.mult)
                else:
                    K.stt(CW[:], CG[:, :, 4 * c:4 * c + 4], OH[:, c:c + 1], CW[:], ALU.mult, ALU.add)
            H = [T("H%d" % i, [128, DC, 512], BF16) for i in range(2)]
            GW_ = [T("GW%d" % i, [128, DC, 256], BF16) for i in range(2)]
            UW_ = [T("UW%d" % i, [128, DC, 256], BF16) for i in range(2)]
            DW_ = [T("DW%d" % i, [128, 8, 512], BF16) for i in range(3)]
            hidT = [T("hidT%d" % i, [128, 8, 512], BF16) for i in range(2)]
            SG = [T("SG%d" % i, [128, 512]) for i in range(2)]
            YP = [T("YP%d" % i, [128, 4, D]) for i in range(2)]
            pgu = [S.ps(p5, "p5_pgu%d" % i, [128, 512]) for i in range(4)]
            pdn = [S.ps(p5, "p5_pdn%d" % i, [128, 512]) for i in range(4)]
            ngu = ndn = nw = ndw = nh = 0
            for j in range(SEQ // 512):
                jc, half = j // 2, j % 2
                Hj = H[j % 2]
                S.dma("sp", [(Hj[:], hg_d[jc * D:(jc + 1) * D, half * 512:(half + 1) * 512].rearrange("(dc p) t -> p dc t", p=128))],
                      "hl%d" % (j % 2))
                yp = YP[j % 2]
                for e in range(4):
                    hid = hidT[nh % 2]
                    nh += 1
                    for piece in range(4):
                        gw, uw = GW_[nw % 2], UW_[nw % 2]
                        S.dma("sp", [(gw[:], wg16_d[e, :, :, piece * 256:(piece + 1) * 256])], "gw%d" % (nw % 2))
                        S.dma("sp", [(uw[:], wu16_d[e, :, :, piece * 256:(piece + 1) * 256])], "uw%d" % (nw % 2))
                        nw += 1
                        for hcl in range(2):
                            hc = piece * 2 + hcl
                            pg_, pu_ = pgu[ngu % 4], pgu[(ngu + 1) % 4]
                            ngu += 2
                            for dc in range(DC):
                                K.mm(pg_[:], gw[:, dc, hcl * 128:(hcl + 1) * 128], Hj[:, dc, :], start=(dc == 0), stop=(dc == DC - 1))
                            for dc in range(DC):
                                K.mm(pu_[:], uw[:, dc, hcl * 128:(hcl + 1) * 128], Hj[:, dc, :], start=(dc == 0), stop=(dc == DC - 1))
                            sg = SG[hc % 2]
                            K.act(sg[:], pg_[:], AF.Silu)
                            K.tt("dve", hid[:, hc, :], sg[:], pu_[:], ALU.mult)
                    for n4 in range(4):
                        dw = DW_[ndw % 3]
                        ndw += 1
                        S.dma("sp", [(dw[:], wd16_d[e, :, :, n4 * 512:(n4 + 1) * 512])], "dw%d" % (ndw % 3))
                        for tb in range(4):
                            pd_ = pdn[ndn % 4]
                            ndn += 1
                            for hc in range(8):
                                K.mm(pd_[:], hid[:, hc, tb * 128:(tb + 1) * 128], dw[:, hc, :], start=(hc == 0), stop=(hc == 7))
                            cw = CW[:, j * 4 + tb, e:e + 1]
                            dst = yp[:, tb, n4 * 512:(n4 + 1) * 512]
                            if e == 0:
                                K.ts("dve", dst, pd_[:], cw, None, ALU.mult)
                            else:
                                K.stt(dst, pd_[:], cw, dst, ALU.mult, ALU.add)
                S.dma("sp", [(yp_d[j * 512:(j + 1) * 512, :].rearrange("(tb p) n -> p tb n", p=128), yp[:])], "yps")
            if L is None:
                S.collective("ReduceScatter", ALU.add, yp_t, ym_t, "rs1")
            S.flush()

    def phase6():
        with ExitStack() as p6:
            T = lambda name, shape, dt=F32: S.sb(p6, "p6_" + name, shape, dt)
            GF = T("GF", [128, D])
            S.dma("sp", [(GF[:], gfin_d.partition_broadcast(128))], "gf")
            xa = [T("xa%d" % i, [128, D]) for i in range(2)]
            ya_ = [T("ya%d" % i, [128, D]) for i in range(2)]
            junk = T("junk", [128, D], BF16)
            ssq = [T("ssq%d" % i, [128, 1]) for i in range(2)]
            fin = []
            for tb in range(TOK_PER_CORE // 128):
                a, b = xa[tb % 2], ya_[tb % 2]
                if L is None:
                    S.dma("sp", [(a[:], x2s_d[tb * 128:(tb + 1) * 128, :]), (b[:], ym_d[tb * 128:(tb + 1) * 128, :])], "f%d" % (tb % 2))
                    K.tt("dve", a[:], a[:], b[:], ALU.add)
                else:
                    S.dma("sp", [(a[:], x2s_d[tb * 128:(tb + 1) * 128, :])], "f%d" % (tb % 2))
                    for c in range(NCORE):
                        S.dma("sp", [(b[:], ypi_d[c, tb * 128:(tb + 1) * 128, :])], "g%d" % (tb % 2))
                        K.tt("dve", a[:], a[:], b[:], ALU.add)
                q = ssq[tb % 2]
                K.act(junk[:], a[:], AF.Square, accum=q[:])
                K.ts("dve", q[:], q[:], 1.0 / D, 1e-6, ALU.mult, ALU.add)
                K.act(q[:], q[:], AF.Sqrt)
                K.recip(q[:], q[:])
                K.act(b[:], a[:], AF.Copy, scale=q[:])
                K.tt("dve", b[:], b[:], GF[:], ALU.mult)
                S.dma("sp", [(out_d[tb * 128:(tb + 1) * 128, :], b[:])], "out")
            S.flush()

    import os
    SKIP123 = bool(os.environ.get("DBG_SKIP123"))
    if upto >= 1 and not SKIP123 and L in (None, 1):
        run_pass(0)
    if upto >= 2 and not SKIP123 and L in (None, 1):
        run_pass(1)
    if upto >= 3 and not SKIP123 and L in (None, 1):
        attention_phase()
    if upto >= 4 and L in (None, 2):
        OH_, gffn_ = phase4()
    if L == 3:
        OH_ = None
        gffn_ = sb("gffn_sb", [128, DC])
        S.dma("sp", [(gffn_[:], gffn_d)], "p4c")
    if upto >= 5 and L in (None, 3):
        phase5(OH_, gffn_)
    if upto >= 6 and L in (None, 4):
        phase6()
    if debug and upto < 4:
        with ExitStack() as dstk:
            dtile = S.sb(dstk, "dbgt", [128, 2048], F32)
            fin = []
            if upto >= 3:
                fin.append(S.dma("sp", [(dbg["yb"][:, :], yg_d[256:512, :])], "dbg0"))
            elif upto >= 2:
                fin.append(S.dma("sp", [(dbg["yb"][0:128, :], ybounce_d[0:128, :])], "dbg0"))
            fin.append(S.dma("sp", [(dbg["yf"], yf_d)], "dbg1"))
            S.flush()
    return nc, K, dict(st=st, att=att, dbg=dbg, yf_d=yf_d, ybounce_d=ybounce_d, yg_t=yg_t, ybounce_t=ybounce_t,
                       AT=AT, alloc_att=alloc_att, run_pass=run_pass, out_d=out_d, cst=cst, sel65=sel65, sb=sb,
                       amask_d=amask_d, x_d=x_d, declared=declared, wout_d=wout_d, wr_d=wr_d, br_d=br_d, gffn_d=gffn_d, gfin_d=gfin_d,
                       wg_d=wg_d, wu_d=wu_d, wd_d=wd_d, identb=identb)


_CACHE = {}
FUSED = False


def _run(launch, ims_all):
    if launch not in _CACHE:
        nc, K, ctx = build(upto=6, debug=False, launch=launch)
        _CACHE[launch] = (nc, ctx["declared"])
    nc, declared = _CACHE[launch]
    ims = [{k: m[k] for k in declared} for m in ims_all]
    return run_bass_kernel_spmd(nc, ims, core_ids=list(range(NCORE))).results


def kernel(**inputs):
    maps = prep_inputs(inputs)
    if FUSED:
        res = _run(None, maps)
        out = np.concatenate([np.asarray(res[c]["out"]) for c in range(NCORE)], axis=0)
        return out.reshape(1, SEQ, D).astype(np.float32)
    r1 = _run(1, maps)
    yg = np.concatenate([np.asarray(r1[c]["ybo"]) for c in range(NCORE)], axis=0)
    for c in range(NCORE):
        maps[c]["yt"] = np.ascontiguousarray(yg[:, c * TOK_PER_CORE:(c + 1) * TOK_PER_CORE])
    r2 = _run(2, maps)
    hg = np.concatenate([np.asarray(r2[c]["hbo"]) for c in range(NCORE)], axis=0)
    cw_all = np.concatenate([np.asarray(r2[c]["cbo"]) for c in range(NCORE)], axis=0)
    for c in range(NCORE):
        maps[c]["hg"] = hg
        maps[c]["cwi"] = np.ascontiguousarray(cw_all[:, 4 * c:4 * c + 4])
    r3 = _run(3, maps)
    for c in range(NCORE):
        maps[c]["ypi"] = np.stack([np.asarray(r3[k]["ypo"])[c * TOK_PER_CORE:(c + 1) * TOK_PER_CORE] for k in range(NCORE)])
        maps[c]["x2i"] = np.asarray(r2[c]["x2o"])
    r4 = _run(4, maps)
    out = np.concatenate([np.asarray(r4[c]["out"]) for c in range(NCORE)], axis=0)
    return out.reshape(1, SEQ, D).astype(np.float32)
```
